# Optimizing a Trainium2 kernel written in Bass

```python
import math
import jax
import jax.numpy as jnp
from jax import lax
import numpy as np

D_MODEL = 2048
BATCH = 1
SEQ = 16384
DEPTH = 4

GRID_W = 64
CTX_LEN = 256
N_MIXERS = 2
N_SSD = (DEPTH + 1) // 2
N_FNET = DEPTH // 2
EXPAND = 2
D_INNER = EXPAND * D_MODEL
HEAD_DIM = 64
N_HEADS = D_INNER // HEAD_DIM
N_GROUPS = 8
HEADS_PER_GROUP = N_HEADS // N_GROUPS
D_STATE = 128
CONV_K = 3
CONV_DIM = D_INNER + 2 * N_GROUPS * D_STATE
IN_DIM = 2 * D_INNER + 2 * N_GROUPS * D_STATE + 2 * N_HEADS
CHUNK = 128
DT_MIN = 1e-3
DT_MAX = 1e-1
FNET_GROUPS = 8
N_EXPERTS = 16
EXPERT_FF = D_MODEL // 2
CAPACITY_FACTOR = 2
EPS = 1e-6

kernel_name = "hybrid_ssd_fnet_ecmoe_prefix_dit"


def rms_norm(x, w):
    xf = x.astype(jnp.float32)
    y = xf * lax.rsqrt(jnp.mean(xf * xf, axis=-1, keepdims=True) + EPS)
    return y.astype(x.dtype) * w


def ada_params(cond, w, b):
    return jnp.split(jax.nn.silu(cond) @ w + b, 6, axis=-1)


def modulate(x, gain, shift, scale):
    return rms_norm(x, gain) * (1 + scale) + shift


def dwconv_grid(u, w, bias, rows):
    b, seq, ch = u.shape
    img = u.reshape(b, rows, GRID_W, ch)
    out = lax.conv_general_dilated(img, w[:, :, None, :], window_strides=(1, 1), padding='SAME',
                                   dimension_numbers=('NHWC', 'HWIO', 'NHWC'), feature_group_count=ch)
    return out.reshape(b, seq, ch) + bias


def dwconv_seq(u, w, bias):
    out = lax.conv_general_dilated(u, w[:, None, :], window_strides=(1,), padding='SAME',
                                   dimension_numbers=('NWC', 'WIO', 'NWC'), feature_group_count=u.shape[-1])
    return out + bias


def ssd_scan(x, dt, a, bm, cm, init):
    f32 = jnp.float32
    b, seq = x.shape[:2]
    nc = seq // CHUNK
    xdt = (x.astype(f32) * dt[..., None]).reshape(b, seq, N_GROUPS, HEADS_PER_GROUP, HEAD_DIM)
    da = (dt * a).reshape(b, seq, N_GROUPS, HEADS_PER_GROUP)

    def chunks(t):
        return jnp.moveaxis(t.reshape((b, nc, CHUNK) + t.shape[2:]), 1, 0)

    inputs = (chunks(xdt), chunks(da), chunks(bm.astype(f32)), chunks(cm.astype(f32)))
    lower = jnp.tril(jnp.ones((CHUNK, CHUNK), dtype=bool))[None, :, :, None, None]

    def step(state, inp):
        xc, ac, bc, cc = inp
        acs = jnp.cumsum(ac, axis=1)
        seg = acs[:, :, None] - acs[:, None, :]
        decay = jnp.exp(jnp.where(lower, seg, -jnp.inf))
        cb = jnp.einsum('blgn,bsgn->blsg', cc, bc)
        y = jnp.einsum('blsg,blsgk,bsgkp->blgkp', cb, decay, xc)
        y = y + jnp.einsum('blgn,bgkpn->blgkp', cc, state) * jnp.exp(acs)[..., None]
        last = acs[:, -1]
        w_in = jnp.exp(last[:, None] - acs)
        state = state * jnp.exp(last)[..., None, None] + jnp.einsum('bsgn,bsgk,bsgkp->bgkpn', bc, w_in, xc)
        return state, y

    init = init.reshape(b, N_GROUPS, HEADS_PER_GROUP, HEAD_DIM, D_STATE)
    final, ys = lax.scan(step, init, inputs)
    y = jnp.moveaxis(ys, 0, 1).reshape(b, seq, N_HEADS, HEAD_DIM)
    return y, final.reshape(b, N_HEADS, HEAD_DIM, D_STATE)


def ssd_project(h, w_in, conv):
    b, seq, _ = h.shape
    zxbcdt = h @ w_in
    z, xbc, dt_raw = jnp.split(zxbcdt, [D_INNER, D_INNER + CONV_DIM], axis=-1)
    xbc = jax.nn.silu(conv(xbc))
    xs, bm, cm = jnp.split(xbc, [D_INNER, D_INNER + N_GROUPS * D_STATE], axis=-1)
    return (z, xs.reshape(b, seq, N_HEADS, HEAD_DIM), bm.reshape(b, seq, N_GROUPS, D_STATE),
            cm.reshape(b, seq, N_GROUPS, D_STATE), dt_raw.reshape(b, seq, 2, N_HEADS))


def ssd_output(y_f, y_b, xs, z, d_skip, norm_w, w_out):
    f32 = jnp.float32
    b, seq = z.shape[:2]
    y = y_f + y_b + xs.astype(f32) * d_skip.astype(f32)[:, None]
    y = y.reshape(b, seq, D_INNER) * jax.nn.silu(z.astype(f32))
    yg = y.reshape(b, seq, N_GROUPS, D_INNER // N_GROUPS)
    yg = yg * lax.rsqrt(jnp.mean(yg * yg, axis=-1, keepdims=True) + EPS)
    y = yg.reshape(b, seq, D_INNER).astype(z.dtype) * norm_w
    return y @ w_out


def ssd_mixer(h_lat, h_ctx, rows, w_in, conv_w, conv_b, dt_bias, a_log, d_skip, norm_w, w_out, ctx_out):
    f32 = jnp.float32
    z_c, x_c, b_c, c_c, dt_c = ssd_project(h_ctx, w_in, lambda u: dwconv_seq(u, conv_w[CONV_K // 2], conv_b))
    z_l, x_l, b_l, c_l, dt_l = ssd_project(h_lat, w_in, lambda u: dwconv_grid(u, conv_w, conv_b, rows))
    a = -jnp.exp(a_log.astype(f32))
    dt_c = jax.nn.softplus(dt_c.astype(f32) + dt_bias.astype(f32))
    dt_l = jax.nn.softplus(dt_l.astype(f32) + dt_bias.astype(f32))
    zero = jnp.zeros((h_lat.shape[0], N_HEADS, HEAD_DIM, D_STATE), f32)
    rev = lambda t: jnp.flip(t, axis=1)
    yc_f, sc_f = ssd_scan(x_c, dt_c[:, :, 0], a[0], b_c, c_c, zero)
    yl_f, _ = ssd_scan(x_l, dt_l[:, :, 0], a[0], b_l, c_l, sc_f)
    yc_b, sc_b = ssd_scan(rev(x_c), rev(dt_c[:, :, 1]), a[1], rev(b_c), rev(c_c), zero)
    yl_b, _ = ssd_scan(rev(x_l), rev(dt_l[:, :, 1]), a[1], rev(b_l), rev(c_l), sc_b)
    out_lat = ssd_output(yl_f, rev(yl_b), x_l, z_l, d_skip, norm_w, w_out)
    out_ctx = ssd_output(yc_f, rev(yc_b), x_c, z_c, d_skip, norm_w, w_out) if ctx_out else None
    return out_lat, out_ctx


def fourier_mixer(h, w_out):
    b, seq, d = h.shape
    hg = h.astype(jnp.float32).reshape(b, seq, FNET_GROUPS, d // FNET_GROUPS)
    f = jnp.fft.fft2(hg, axes=(1, 3), norm='ortho').real
    return f.reshape(b, seq, d).astype(h.dtype) @ w_out


def ec_moe(h, w_router, w_gate, w_up, w_down):
    n = h.shape[1]
    cap = (CAPACITY_FACTOR * n) // N_EXPERTS
    aff = jax.nn.softmax(jnp.einsum('bnd,de->bne', h, w_router).astype(jnp.float32), axis=-1)
    g, idx = lax.top_k(jnp.swapaxes(aff, 1, 2), cap)

    def per_sample(h_b, g_b, idx_b):
        xs = h_b[idx_b]
        a = jnp.einsum('ecd,edf->ecf', xs, w_gate)
        u = jnp.einsum('ecd,edf->ecf', xs, w_up)
        y = jnp.einsum('ecf,efd->ecd', jax.nn.silu(a) * u, w_down) * g_b[..., None].astype(h_b.dtype)
        return jnp.zeros_like(h_b).at[idx_b.reshape(-1)].add(y.reshape(-1, h_b.shape[-1]))

    return jax.vmap(per_sample)(h, g, idx)


def setup_inputs(seed: int = 0) -> dict:
    key = jax.random.key(seed)
    ks = jax.random.split(key, 24)
    f32 = jnp.float32
    nrm = lambda k, shape, s: jax.random.normal(k, shape, f32) * s
    x = nrm(ks[0], (BATCH, SEQ, D_MODEL), 1.0)
    c = nrm(ks[1], (BATCH, D_MODEL), 1.0)
    ctx = nrm(ks[2], (BATCH, CTX_LEN, D_MODEL), 1.0)
    c_ctx = nrm(ks[3], (D_MODEL,), 1.0)
    ada_w = nrm(ks[4], (DEPTH, D_MODEL, 6 * D_MODEL), 0.5 * D_MODEL ** -0.5)
    ada_b = nrm(ks[5], (DEPTH, 6 * D_MODEL), 0.02)
    norm1_w = 1.0 + nrm(ks[6], (DEPTH, D_MODEL), 0.05)
    norm2_w = 1.0 + nrm(ks[7], (DEPTH, D_MODEL), 0.05)
    final_norm_w = 1.0 + nrm(ks[8], (D_MODEL,), 0.05)
    ssd_w_in = nrm(ks[9], (N_SSD, D_MODEL, IN_DIM), D_MODEL ** -0.5)
    ssd_conv_w = nrm(ks[10], (N_SSD, CONV_K, CONV_K, CONV_DIM), 1.0 / CONV_K)
    ssd_conv_b = nrm(ks[11], (N_SSD, CONV_DIM), 0.02)
    dt0 = jnp.exp(jax.random.uniform(ks[12], (N_SSD, 2, N_HEADS), f32, math.log(DT_MIN), math.log(DT_MAX)))
    ssd_dt_bias = dt0 + jnp.log(-jnp.expm1(-dt0))
    ssd_a_log = jnp.log(jax.random.uniform(ks[13], (N_SSD, 2, N_HEADS), f32, 1.0, 16.0))
    ssd_d = 1.0 + nrm(ks[14], (N_SSD, N_HEADS), 0.05)
    ssd_norm_w = 1.0 + nrm(ks[15], (N_SSD, D_INNER), 0.05)
    ssd_w_out = nrm(ks[16], (N_SSD, D_INNER, D_MODEL), D_INNER ** -0.5)
    fnet_w_out = nrm(ks[17], (N_FNET, D_MODEL, D_MODEL), D_MODEL ** -0.5)
    moe_w_router = nrm(ks[18], (DEPTH, D_MODEL, N_EXPERTS), D_MODEL ** -0.5)
    moe_w_gate = nrm(ks[19], (DEPTH, N_EXPERTS, D_MODEL, EXPERT_FF), D_MODEL ** -0.5)
    moe_w_up = nrm(ks[20], (DEPTH, N_EXPERTS, D_MODEL, EXPERT_FF), D_MODEL ** -0.5)
    moe_w_down = nrm(ks[21], (DEPTH, N_EXPERTS, EXPERT_FF, D_MODEL), EXPERT_FF ** -0.5)
    return {"x": x, "c": c, "ctx": ctx, "c_ctx": c_ctx, "ada_w": ada_w, "ada_b": ada_b,
            "norm1_w": norm1_w, "norm2_w": norm2_w, "final_norm_w": final_norm_w,
            "ssd_w_in": ssd_w_in, "ssd_conv_w": ssd_conv_w, "ssd_conv_b": ssd_conv_b,
            "ssd_dt_bias": ssd_dt_bias, "ssd_a_log": ssd_a_log, "ssd_d": ssd_d,
            "ssd_norm_w": ssd_norm_w, "ssd_w_out": ssd_w_out, "fnet_w_out": fnet_w_out,
            "moe_w_router": moe_w_router, "moe_w_gate": moe_w_gate, "moe_w_up": moe_w_up,
            "moe_w_down": moe_w_down}


def reference(x, c, ctx, c_ctx, ada_w, ada_b, norm1_w, norm2_w, final_norm_w, ssd_w_in, ssd_conv_w,
              ssd_conv_b, ssd_dt_bias, ssd_a_log, ssd_d, ssd_norm_w, ssd_w_out, fnet_w_out,
              moe_w_router, moe_w_gate, moe_w_up, moe_w_down):
    rows = x.shape[1] // GRID_W
    ctx_s = ctx
    for i in range(DEPTH):
        k = i // N_MIXERS
        is_ssd = (i % N_MIXERS == 0)
        ctx_later = any(j % N_MIXERS == 0 for j in range(i + 1, DEPTH))
        sh1, sc1, g1, sh2, sc2, g2 = ada_params(c[:, None, :], ada_w[i], ada_b[i])
        h = modulate(x, norm1_w[i], sh1, sc1)
        if is_ssd or ctx_later:
            csh1, csc1, cg1, csh2, csc2, cg2 = ada_params(c_ctx[None, None, :], ada_w[i], ada_b[i])
            hc = modulate(ctx_s, norm1_w[i], csh1, csc1)
        if is_ssd:
            y, yc = ssd_mixer(h, hc, rows, ssd_w_in[k], ssd_conv_w[k], ssd_conv_b[k], ssd_dt_bias[k],
                              ssd_a_log[k], ssd_d[k], ssd_norm_w[k], ssd_w_out[k], ctx_later)
        else:
            y = fourier_mixer(h, fnet_w_out[k])
            yc = fourier_mixer(hc, fnet_w_out[k]) if ctx_later else None
        x = x + g1 * y
        x = x + g2 * ec_moe(modulate(x, norm2_w[i], sh2, sc2), moe_w_router[i], moe_w_gate[i],
                            moe_w_up[i], moe_w_down[i])
        if ctx_later:
            ctx_s = ctx_s + cg1 * yc
            ctx_s = ctx_s + cg2 * ec_moe(modulate(ctx_s, norm2_w[i], csh2, csc2), moe_w_router[i],
                                         moe_w_gate[i], moe_w_up[i], moe_w_down[i])
    return rms_norm(x, final_norm_w)
```

```python
import math
import numpy as np
import ml_dtypes
import concourse.bass as bass
import concourse.mybir as mybir
from concourse.bass_utils import run_bass_kernel_spmd

F32, BF16, I32 = mybir.dt.float32, mybir.dt.bfloat16, mybir.dt.int32
ALU = mybir.AluOpType
AF = mybir.ActivationFunctionType
AX = mybir.AxisListType

D = 2048
SEQ = 16384
CTX = 256
NTOK = SEQ + CTX
NT = NTOK // 128
NLT = SEQ // 128
DEPTH = 4
DIN = 4096
NG = 8
HPG = 8
HD = 64
DS = 128
GC = 1296
NE = 16
FF = 1024
CAP = 2048
CAPC = 32
EPS = 1e-6
BIG = 1.0e6


class Tok:
    __slots__ = ("sem", "val", "eng")

    def __init__(self, sem, val, eng):
        self.sem, self.val, self.eng = sem, val, eng


class DSem:
    def __init__(self, h):
        self.h, self.cnt = h, 0


class Buf:
    def __init__(self, name, ds=None):
        self.name = name
        self.w = None
        self.r = {}
        self.ds = ds
        self.excl = False


class K:
    ENGS = ("pe", "act", "dve", "pool", "sp")

    def __init__(self, nc):
        self.nc = nc
        self.E = {"pe": nc.tensor, "act": nc.scalar, "dve": nc.vector, "pool": nc.gpsimd, "sp": nc.sync}
        self.sem = {e: nc.alloc_semaphore("eng_" + e) for e in self.ENGS}
        self.cnt = {e: 0 for e in self.ENGS}
        self.seen = {e: {} for e in self.ENGS}
        self.dsems = []
        self.free_ds = []
        self.inputs = {}
        self.nbuf = 0
        self.ds_owners = []
        self.retired = []
        self.nrot = 0

    def buf(self, name=None):
        self.nbuf += 1
        return Buf(name or "b%d" % self.nbuf)

    def get_ds(self, b):
        if b.ds is None:
            if self.free_ds:
                b.ds = self.free_ds.pop()
            else:
                self.nds = getattr(self, "nds", 0) + 1
                b.ds = DSem(self.nc.alloc_semaphore("ds%d" % self.nds))
                self.dsems.append(b.ds)
            self.ds_owners.append(b)
        return b.ds

    def release(self, bufs):
        for b in bufs:
            if b.ds is not None:
                self.free_ds.append(b.ds)
                b.ds = None

    def wait(self, eng, t):
        s = self.seen[eng]
        key = id(t.sem)
        if s.get(key, 0) >= t.val:
            return
        self.E[eng].wait_ge(t.sem, t.val)
        s[key] = t.val

    def _deps(self, eng, reads, writes, is_dma):
        for b in reads:
            if b.w is not None:
                self.wait(eng, b.w)
        for b in writes:
            if b.w is not None:
                if b.w.eng == "dma":
                    if not is_dma:
                        self.wait(eng, b.w)
                elif b.w.eng != eng or is_dma:
                    self.wait(eng, b.w)
            for t in b.r.values():
                if t.eng == eng and not is_dma:
                    continue
                self.wait(eng, t)

    def _commit(self, tok, reads, writes):
        for b in reads:
            b.r[id(tok.sem)] = tok
        for b in writes:
            b.w = tok
            b.r = {}

    def op(self, eng, fn, reads=(), writes=(), sig=True):
        ex = [b for b in reads if b.excl]
        if ex:
            reads = [b for b in reads if not b.excl]
            writes = list(writes) + ex
        self._deps(eng, reads, writes, False)
        ins = fn(self.E[eng])
        if sig:
            self.cnt[eng] += 1
            ins.then_inc(self.sem[eng], 1)
            tok = Tok(self.sem[eng], self.cnt[eng], eng)
        else:
            tok = Tok(self.sem[eng], self.cnt[eng] + 1, eng)
        self._commit(tok, reads, writes)
        return tok

    def dma(self, q, out, in_, reads=(), writes=(), **kw):
        self._deps(q, reads, writes, True)
        ins = self.E[q].dma_start(out=out, in_=in_, **kw)
        ds = self.get_ds(writes[0])
        ds.cnt += 16
        ins.then_inc(ds.h, 16)
        tok = Tok(ds.h, ds.cnt, "dma")
        self._commit(tok, reads, writes)
        return tok

    def idma(self, out, out_off, in_, in_off, reads=(), writes=(), **kw):
        q = "pool"
        if "bounds_check" in kw and isinstance(kw["bounds_check"], int):
            v = kw["bounds_check"]
            if not hasattr(self, "bregs"):
                self.bregs = {}
            if v not in self.bregs:
                self.bregs[v] = self.nc.gpsimd.to_reg(v)
            kw["bounds_check"] = self.bregs[v]
        self._deps(q, reads, writes, True)
        ins = self.nc.gpsimd.indirect_dma_start(out=out, out_offset=out_off, in_=in_, in_offset=in_off, **kw)
        ds = self.get_ds(writes[0])
        ds.cnt += 16
        ins.then_inc(ds.h, 16)
        tok = Tok(ds.h, ds.cnt, "dma")
        self._commit(tok, reads, writes)
        return tok

    def barrier(self):
        for e in self.ENGS:
            for o in self.ENGS:
                if o != e and self.cnt[o] > 0:
                    self.wait(e, Tok(self.sem[o], self.cnt[o], o))
            for ds in self.dsems:
                if ds.cnt > 0:
                    self.wait(e, Tok(ds.h, ds.cnt, "dma"))
        for b in self.ds_owners:
            if b.ds is not None:
                if b.ds not in self.free_ds:
                    self.free_ds.append(b.ds)
                b.ds = None
        self.ds_owners = []
        for ds in list(self.dsems):
            if ds.cnt > 36000:
                self.dsems.remove(ds)
                if ds in self.free_ds:
                    self.free_ds.remove(ds)
                self.retired.append(ds)
        for e in self.ENGS:
            if self.cnt[e] > 28000:
                self.retired.append(self.sem[e])
                self.nrot += 1
                self.sem[e] = self.nc.alloc_semaphore("eng_%s_%d" % (e, self.nrot))
                self.cnt[e] = 0

    def inp(self, name, shape, dt=F32):
        if name not in self.inputs:
            self.inputs[name] = self.nc.dram_tensor(name, list(shape), dt, kind="ExternalInput")
        return self.inputs[name]


def bview(ap2d, parts=128):
    return ap2d.partition_broadcast(parts)


def host_consts():
    c = {}
    c["ident_f"] = np.eye(128, dtype=np.float32)
    c["ident_b"] = np.eye(128, dtype=np.float32).astype(ml_dtypes.bfloat16)
    k = np.arange(128)
    tri = np.zeros((128, 5, 128), np.float32)
    tri[:, 0, :] = (k[:, None] <= k[None, :])
    tri[:, 1, :] = (k[:, None] > k[None, :])
    tri[:, 2, :] = (k[:, None] >= k[None, :])
    tri[:, 3, :] = (k[:, None] < k[None, :])
    tri[:, 4, :] = 1.0
    c["tri"] = tri
    msk = np.zeros((128, 2, 128), np.float32)
    msk[:, 0, :] = (k[None, :] >= k[:, None])
    msk[:, 1, :] = (k[None, :] <= k[:, None])
    c["msk"] = msk
    n = np.arange(128, dtype=np.float64)
    a128 = 2 * np.pi * np.outer(n, n) / 128.0
    fc, fs = np.cos(a128), np.sin(a128)
    f1 = np.zeros((128, 2, 256), np.float64)
    f1[:, 0, :128], f1[:, 0, 128:] = fc, -fs
    f1[:, 1, :128], f1[:, 1, 128:] = fs, fc
    c["f1"] = f1.astype(ml_dtypes.bfloat16)
    f2 = np.zeros((128, 2, 128), np.float64)
    f2[:, 0, :], f2[:, 1, :] = fc, fs
    c["f2"] = f2.astype(ml_dtypes.bfloat16)
    m = np.arange(256, dtype=np.float64)
    a256 = 2 * np.pi * np.outer(m, m) / 256.0
    cs = np.zeros((128, 2, 2, 256), np.float64)
    c256, s256 = np.cos(a256), np.sin(a256)
    for t in range(2):
        cs[:, t, 0, :] = c256[t * 128:(t + 1) * 128, :]
        cs[:, t, 1, :] = -s256[t * 128:(t + 1) * 128, :]
    c["cs256"] = cs.astype(ml_dtypes.bfloat16)
    ps = np.zeros((128, 2, 2, 256), np.float64)
    for t in range(2):
        ps[:, t, 0, :] = c256[t * 128:(t + 1) * 128, :]
        ps[:, t, 1, :] = s256[t * 128:(t + 1) * 128, :]
    c["ps256"] = ps.astype(ml_dtypes.bfloat16)
    at = 2 * np.pi * np.outer(n, n) / 16384.0
    wr, wi = np.cos(at), -np.sin(at)
    tw = np.zeros((128, 2, 2, 128), np.float32)
    tw[:, 0, 0, :], tw[:, 0, 1, :] = wr, wi
    tw[:, 1, 0, :], tw[:, 1, 1, :] = wi, wr
    c["tw"] = tw
    tid = (np.arange(NT)[None, :] * 128 + np.arange(128)[:, None]).astype(np.int32)
    c["tid"] = tid
    c["oob"] = np.full((128, 64), 1 << 24, np.int32)
    return c


CONST_SPECS = {
    "ident_f": ([128, 128], F32), "ident_b": ([128, 128], BF16), "tri": ([128, 5, 128], F32),
    "msk": ([128, 2, 128], F32), "f1": ([128, 2, 256], BF16), "f2": ([128, 2, 128], BF16),
    "cs256": ([128, 2, 2, 256], BF16), "ps256": ([128, 2, 2, 256], BF16), "tw": ([128, 2, 2, 128], F32),
    "tid": ([128, NT], I32), "oob": ([128, 64], I32),
}


from contextlib import ExitStack


class Prog:
    def __init__(self, debug_outs=()):
        self.nc = bass.Bass(target_bir_lowering=False)
        nc = self.nc
        self.k = K(nc)
        self.st = ExitStack()
        self.debug_outs = set(debug_outs)
        self.dram = {}
        self.dbuf = {}
        self.ps = [self.st.enter_context(nc.psum_tensor("psb%d" % i, [128, 512], F32)) for i in range(8)]
        self.psb = [self.k.buf("ps%d" % i) for i in range(8)]
        for b in self.psb:
            b.excl = True
        self.c = {}
        self.cb = {}

    def scratch(self, name, shape, dt):
        if name not in self.dram:
            kind = "ExternalOutput" if name in self.debug_outs else "Internal"
            self.dram[name] = self.nc.dram_tensor(name, list(shape), dt, kind=kind)
            self.dbuf[name] = self.k.buf("d_" + name)
        return self.dram[name]

    def sb(self, st, name, shape, dt):
        self.nsb = getattr(self, "nsb", 0) + 1
        return st.enter_context(self.nc.sbuf_tensor("%s_u%d" % (name, self.nsb), list(shape), dt))

    def load_const(self, name, st=None):
        if st is None and name in self.c:
            return
        shape, dt = CONST_SPECS[name]
        src = self.k.inp("c_" + name, shape, dt)
        self.nconst = getattr(self, "nconst", 0) + 1
        t = self.sb(st if st is not None else self.st, "sc%d_" % self.nconst + name, shape, dt)
        b = self.k.buf("c_" + name)
        self.k.dma("sp", t.ap(), src.ap(), writes=[b])
        self.c[name], self.cb[name] = t, b

    def phase_init(self, from_xs=False):
        k = self.k
        xs = self.scratch("xs", [NTOK, D], F32)
        b = self.dbuf["xs"]
        if from_xs:
            xi = k.inp("xs_in", [NTOK, D])
            for i in range(10):
                k.dma("sp", xs[i * 1664:(i + 1) * 1664, :], xi[i * 1664:(i + 1) * 1664, :], writes=[b])
            return
        x = k.inp("x", [SEQ, D])
        ctx = k.inp("ctx", [CTX, D])
        for i in range(8):
            k.dma("sp", xs[i * 2048:(i + 1) * 2048, :], x[i * 2048:(i + 1) * 2048, :], writes=[b])
        k.dma("sp", xs[SEQ:NTOK, :], ctx[:, :], writes=[b])

    def phase_dump_xs(self):
        k = self.k
        xs, xb = self.dram["xs"], self.dbuf["xs"]
        xo = self.nc.dram_tensor("xs_out", [NTOK, D], F32, kind="ExternalOutput")
        ob = k.buf("xs_out")
        for i in range(10):
            k.dma("sp", xo[i * 1664:(i + 1) * 1664, :], xs[i * 1664:(i + 1) * 1664, :], reads=[xb], writes=[ob])
        k.barrier()

    def phase_ada(self, layers):
        k, nc = self.k, self.nc
        mod = self.scratch("mod", [DEPTH, 2, 6 * D], F32)
        mb = self.dbuf["mod"]
        cin = k.inp("c", [1, D])
        cc = k.inp("c_ctx", [1, D])
        with ExitStack() as st:
            cs = self.sb(st, "ada_cs", [128, 16, 2], F32)
            cT = self.sb(st, "ada_cT", [128, 16, 2], F32)
            wb = [self.sb(st, "ada_w%d" % i, [128, 16, 512], F32) for i in range(2)]
            bb = [self.sb(st, "ada_b%d" % i, [2, 512], F32) for i in range(2)]
            ob = [self.sb(st, "ada_o%d" % i, [2, 512], F32) for i in range(2)]
            b_cs, b_cT = k.buf(), k.buf()
            b_w = [k.buf(), k.buf()]
            b_b = [k.buf(), k.buf()]
            b_o = [k.buf(), k.buf()]
            with nc.allow_non_contiguous_dma(reason="tiny transposed cond load"):
                k.dma("sp", cs[:, :, 0], cin.ap().rearrange("o (t p) -> p (o t)", p=128), writes=[b_cs])
                k.dma("sp", cs[:, :, 1], cc.ap().rearrange("o (t p) -> p (o t)", p=128), writes=[b_cs])
            k.op("act", lambda e: e.activation(out=cT[:, :, :], in_=cs[:, :, :], func=AF.Silu), reads=[b_cs], writes=[b_cT])
            it = 0
            for l in layers:
                aw = k.inp("ada_w_%d" % l, [D, 6 * D])
                ab = k.inp("ada_b_%d" % l, [1, 6 * D])
                awv = aw.ap().rearrange("(t p) n -> p t n", p=128)
                for j in range(24):
                    s = it % 2
                    it += 1
                    k.dma("sp", wb[s][:, :, :], awv[:, :, j * 512:(j + 1) * 512], writes=[b_w[s]])
                    k.dma("sp", bb[s][:, :], bview(ab[0:1, j * 512:(j + 1) * 512], 2), writes=[b_b[s]])
                    pb = self.psb[s]
                    for t in range(16):
                        k.op("pe", lambda e, t=t, s=s: e.matmul(self.ps[s][0:2, :], lhsT=cT[:, t, :], rhs=wb[s][:, t, :],
                                                             start=(t == 0), stop=(t == 15)),
                             reads=[b_cT, b_w[s]], writes=[pb], sig=(t == 15))
                    k.op("dve", lambda e, s=s: e.tensor_tensor(out=ob[s][:, :], in0=self.ps[s][0:2, :], in1=bb[s][:, :], op=ALU.add),
                         reads=[pb, b_b[s]], writes=[b_o[s]])
                    k.dma("sp", mod[l, :, j * 512:(j + 1) * 512], ob[s][:, :], reads=[b_o[s]], writes=[mb])
        k.barrier()

    def load_mod(self, st, l, which, tag):
        k = self.k
        mod = self.dram["mod"]
        mb = self.dbuf["mod"]
        nw = k.inp("norm%d_w" % which, [DEPTH, D])
        o_sh = 0 if which == 1 else 3 * D
        o_sc = o_sh + D
        res = {}
        nwt = self.sb(st, tag + "nw", [128, D], F32)
        b_nw = k.buf()
        k.dma("sp", nwt[:, :], bview(nw[l:l + 1, :]), writes=[b_nw])
        for r in range(2):
            A = self.sb(st, tag + "A%d" % r, [128, D], F32)
            sh = self.sb(st, tag + "sh%d" % r, [128, D], F32)
            bA, bs = k.buf(), k.buf()
            k.dma("sp", A[:, :], bview(mod[l, r:r + 1, o_sc:o_sc + D]), reads=[mb], writes=[bA])
            k.dma("sp", sh[:, :], bview(mod[l, r:r + 1, o_sh:o_sh + D]), reads=[mb], writes=[bs])
            k.op("dve", lambda e, A=A: e.scalar_tensor_tensor(out=A[:, :], in0=A[:, :], scalar=1.0, in1=nwt[:, :],
                                                           op0=ALU.add, op1=ALU.mult), reads=[bA, b_nw], writes=[bA])
            res[r] = (A, sh, bA, bs)
        return res

    def load_gate(self, st, l, which, tag):
        k = self.k
        mod = self.dram["mod"]
        mb = self.dbuf["mod"]
        off = 2 * D if which == 1 else 5 * D
        res = {}
        for r in range(2):
            g = self.sb(st, tag + "g%d" % r, [128, D], F32)
            bg = k.buf()
            k.dma("sp", g[:, :], bview(mod[l, r:r + 1, off:off + D]), reads=[mb], writes=[bg])
            res[r] = (g, bg)
        return res

    def rstd(self, xt, b_x, junk, b_j, ss, b_ss, rs, b_rs, width=D):
        k = self.k
        k.op("act", lambda e: e.activation(out=junk[:, :], in_=xt, func=AF.Square, accum_out=ss[:, :]),
             reads=[b_x], writes=[b_j, b_ss])
        k.op("act", lambda e: e.activation(out=rs[:, :], in_=ss[:, :], func=AF.Sqrt, scale=1.0 / width, bias=EPS),
             reads=[b_ss], writes=[b_rs])
        k.op("dve", lambda e: e.reciprocal(out=rs[:, :], in_=rs[:, :]), reads=[b_rs], writes=[b_rs])

    def phase_mod_mix(self, l):
        k, nc = self.k, self.nc
        xs = self.dram["xs"]
        xb = self.dbuf["xs"]
        hT = self.scratch("hT", [D, NTOK], BF16)
        hb_ = self.dbuf["hT"]
        self.load_const("ident_b")
        idb, b_id = self.c["ident_b"], self.cb["ident_b"]
        hTv = hT.ap().rearrange("(kt p) n -> p kt n", p=128)
        with ExitStack() as st:
            md = self.load_mod(st, l, 1, "mm_")
            xt = [self.sb(st, "mm_x%d" % i, [128, D], F32) for i in range(2)]
            hn = [self.sb(st, "mm_hn%d" % i, [128, D], F32) for i in range(2)]
            hb = [self.sb(st, "mm_hb%d" % i, [128, D], BF16) for i in range(2)]
            junk = self.sb(st, "mm_junk", [128, D], BF16)
            ss = [self.sb(st, "mm_ss%d" % i, [128, 1], F32) for i in range(2)]
            rs = [self.sb(st, "mm_rs%d" % i, [128, 1], F32) for i in range(2)]
            hTt = [self.sb(st, "mm_hT%d" % i, [128, 16, 512], BF16) for i in range(2)]
            b_x, b_hn, b_hb = [k.buf(), k.buf()], [k.buf(), k.buf()], [k.buf(), k.buf()]
            b_j = k.buf()
            b_ss, b_rs = [k.buf(), k.buf()], [k.buf(), k.buf()]
            b_hT = [k.buf(), k.buf()]
            ngroups = (NT + 3) // 4
            for g in range(ngroups):
                gs = g % 2
                tiles = list(range(g * 4, min(NT, g * 4 + 4)))
                for j, i in enumerate(tiles):
                    s = i % 2
                    r = 0 if i < NLT else 1
                    A, sh, bA, bs = md[r]
                    k.dma("sp", xt[s][:, :], xs[i * 128:(i + 1) * 128, :], reads=[xb], writes=[b_x[s]])
                    self.rstd(xt[s][:, :], b_x[s], junk, b_j, ss[s], b_ss[s], rs[s], b_rs[s])
                    k.op("dve", lambda e, s=s, A=A: e.scalar_tensor_tensor(out=hn[s][:, :], in0=xt[s][:, :], scalar=rs[s][:, 0:1],
                                                                      in1=A[:, :], op0=ALU.mult, op1=ALU.mult),
                         reads=[b_x[s], b_rs[s], bA], writes=[b_hn[s]])
                    k.op("pool", lambda e, s=s, sh=sh: e.tensor_tensor(out=hb[s][:, :], in0=hn[s][:, :], in1=sh[:, :], op=ALU.add),
                         reads=[b_hn[s], bs], writes=[b_hb[s]])
                    for half in range(2):
                        bank = 2 * s + half
                        pv = self.ps[bank][:, :].bitcast(BF16)
                        for q in range(8):
                            kt = half * 8 + q
                            k.op("pe", lambda e, pv=pv, q=q, kt=kt, s=s: e.transpose(out=pv[:, q * 128:(q + 1) * 128],
                                                                                 in_=hb[s][:, kt * 128:(kt + 1) * 128], identity=idb[:, :]),
                                 reads=[b_hb[s], b_id], writes=[self.psb[bank]], sig=(q == 7))
                        eng = "act" if half == 0 else "dve"
                        dst = hTt[gs][:, half * 8:(half + 1) * 8, j * 128:(j + 1) * 128]
                        src = pv.rearrange("p (q n) -> p q n", q=8)
                        if eng == "act":
                            k.op("act", lambda e, dst=dst, src=src: e.copy(out=dst, in_=src), reads=[self.psb[bank]], writes=[b_hT[gs]])
                        else:
                            k.op("dve", lambda e, dst=dst, src=src: e.tensor_copy(out=dst, in_=src), reads=[self.psb[bank]], writes=[b_hT[gs]])
                t0 = tiles[0] * 128
                nt = len(tiles) * 128
                k.dma("sp", hTv[:, :, t0:t0 + nt], hTt[gs][:, :, 0:nt], reads=[b_hT[gs]], writes=[hb_])
        k.barrier()

    def phase_final(self):
        k, nc = self.k, self.nc
        xs = self.dram["xs"]
        xb = self.dbuf["xs"]
        out = nc.dram_tensor("out", [SEQ, D], F32, kind="ExternalOutput")
        ob = k.buf("out")
        fw = k.inp("final_norm_w", [1, D])
        with ExitStack() as st:
            wt = self.sb(st, "fn_w", [128, D], F32)
            b_w = k.buf()
            k.dma("sp", wt[:, :], bview(fw[0:1, :]), writes=[b_w])
            xt = [self.sb(st, "fn_x%d" % i, [128, D], F32) for i in range(2)]
            ot = [self.sb(st, "fn_o%d" % i, [128, D], F32) for i in range(2)]
            junk = self.sb(st, "fn_junk", [128, D], BF16)
            ss = [self.sb(st, "fn_ss%d" % i, [128, 1], F32) for i in range(2)]
            rs = [self.sb(st, "fn_rs%d" % i, [128, 1], F32) for i in range(2)]
            b_x, b_o = [k.buf(), k.buf()], [k.buf(), k.buf()]
            b_j = k.buf()
            b_ss, b_rs = [k.buf(), k.buf()], [k.buf(), k.buf()]
            for i in range(NLT):
                s = i % 2
                k.dma("sp", xt[s][:, :], xs[i * 128:(i + 1) * 128, :], reads=[xb], writes=[b_x[s]])
                self.rstd(xt[s][:, :], b_x[s], junk, b_j, ss[s], b_ss[s], rs[s], b_rs[s])
                k.op("dve", lambda e, s=s: e.scalar_tensor_tensor(out=ot[s][:, :], in0=xt[s][:, :], scalar=rs[s][:, 0:1],
                                                              in1=wt[:, :], op0=ALU.mult, op1=ALU.mult),
                     reads=[b_x[s], b_rs[s], b_w], writes=[b_o[s]])
                k.dma("sp", out[i * 128:(i + 1) * 128, :], ot[s][:, :], reads=[b_o[s]], writes=[ob])
        k.barrier()


def host_input(name, inp, hc):
    if name.startswith("c_") and name[2:] in hc:
        return hc[name[2:]]
    if name == "x":
        return np.ascontiguousarray(inp["x"][0])
    if name == "ctx":
        return np.ascontiguousarray(inp["ctx"][0])
    if name == "c":
        return np.ascontiguousarray(inp["c"].reshape(1, D))
    if name == "c_ctx":
        return np.ascontiguousarray(inp["c_ctx"].reshape(1, D))
    if name == "final_norm_w":
        return np.ascontiguousarray(inp["final_norm_w"].reshape(1, D))
    if name in ("norm1_w", "norm2_w"):
        return np.ascontiguousarray(inp[name])
    for base in ("ada_w", "ada_b", "ssd_w_in", "ssd_conv_w", "ssd_conv_b", "ssd_dt_bias", "ssd_a_log", "ssd_d",
                 "ssd_norm_w", "ssd_w_out", "fnet_w_out", "moe_w_router", "moe_w_gate", "moe_w_up", "moe_w_down"):
        if name.startswith(base + "_") and name[len(base) + 1:].isdigit():
            i = int(name[len(base) + 1:])
            a = inp[base][i]
            if base == "ada_b":
                a = a.reshape(1, -1)
            if base == "ssd_conv_w":
                a = a.reshape(9, -1)
            if base == "ssd_conv_b":
                a = a.reshape(1, -1)
            if base in ("ssd_dt_bias", "ssd_a_log"):
                a = a.reshape(1, 128)
            if base in ("ssd_d",):
                a = a.reshape(1, 64)
            if base == "ssd_norm_w":
                a = a.reshape(1, DIN)
            return np.ascontiguousarray(a)
    raise KeyError(name)


def phase_mod_moe(self, l):
    k, nc = self.k, self.nc
    xs, xb = self.dram["xs"], self.dbuf["xs"]
    h2 = self.scratch("h2", [NTOK, D], BF16)
    h2b = self.dbuf["h2"]
    mo = self.scratch("moe_out", [NTOK, D], F32)
    mob = self.dbuf["moe_out"]
    self.load_const("ident_f")
    idf, b_idf = self.c["ident_f"], self.cb["ident_f"]
    if not hasattr(self, "aff_all"):
        self.aff_all = self.sb(self.st, "aff_all", [128, NT, NE], F32)
        self.b_aff = k.buf("aff_all")
    aff_all, b_aff = self.aff_all, self.b_aff
    wr_d = k.inp("moe_w_router_%d" % l, [D, NE])
    with ExitStack() as st:
        zt = self.sb(st, "mz_zero", [128, 4096], F32)
        b_z = k.buf()
        k.op("pool", lambda e: e.memset(zt[:, :], 0.0), writes=[b_z])
        mov = mo.ap().rearrange("(p a) d -> p (a d)", p=128)
        for j in range(NT * D // 4096):
            k.dma("sp", mov[:, j * 4096:(j + 1) * 4096], zt[:, :], reads=[b_z], writes=[mob])
        md = self.load_mod(st, l, 2, "mz_")
        wr = self.sb(st, "mz_wr", [128, 16, NE], F32)
        b_wr = k.buf()
        with nc.allow_non_contiguous_dma(reason="router weight 64B rows"):
            k.dma("sp", wr[:, :, :], wr_d.ap().rearrange("(kt p) e -> p kt e", p=128), writes=[b_wr])
        xt = [self.sb(st, "mz_x%d" % i, [128, D], F32) for i in range(2)]
        hn = [self.sb(st, "mz_hn%d" % i, [128, D], F32) for i in range(2)]
        hf = [self.sb(st, "mz_hf%d" % i, [128, D], F32) for i in range(2)]
        hb = [self.sb(st, "mz_hb%d" % i, [128, D], BF16) for i in range(2)]
        hfT = [self.sb(st, "mz_hfT%d" % i, [128, 16, 128], F32) for i in range(2)]
        junk = self.sb(st, "mz_junk", [128, D], BF16)
        sm = [self.sb(st, "mz_sm%d" % i, [128, 8], F32) for i in range(2)]
        ex = [self.sb(st, "mz_ex%d" % i, [128, NE], F32) for i in range(2)]
        b_x, b_hn, b_hf, b_hb, b_hfT = ([k.buf(), k.buf()] for _ in range(5))
        b_j = k.buf()
        b_ss, b_rs, b_mx, b_nmx, b_se, b_rse, b_ex = ([k.buf(), k.buf()] for _ in range(7))
        for i in range(NT):
            s = i % 2
            r = 0 if i < NLT else 1
            A, sh, bA, bs = md[r]
            k.dma("sp", xt[s][:, :], xs[i * 128:(i + 1) * 128, :], reads=[xb], writes=[b_x[s]])
            ss, rs = sm[s][:, 0:1], sm[s][:, 1:2]
            k.op("act", lambda e, s=s, ss=ss: e.activation(out=junk[:, :], in_=xt[s][:, :], func=AF.Square, accum_out=ss),
                 reads=[b_x[s]], writes=[b_j, b_ss[s]])
            k.op("act", lambda e, ss=ss, rs=rs: e.activation(out=rs, in_=ss, func=AF.Sqrt, scale=1.0 / D, bias=EPS),
                 reads=[b_ss[s]], writes=[b_rs[s]])
            k.op("dve", lambda e, rs=rs: e.reciprocal(out=rs, in_=rs), reads=[b_rs[s]], writes=[b_rs[s]])
            k.op("dve", lambda e, s=s, A=A, rs=rs: e.scalar_tensor_tensor(out=hn[s][:, :], in0=xt[s][:, :], scalar=rs, in1=A[:, :],
                                                                     op0=ALU.mult, op1=ALU.mult),
                 reads=[b_x[s], b_rs[s], bA], writes=[b_hn[s]])
            k.op("pool", lambda e, s=s, sh=sh: e.tensor_tensor(out=hf[s][:, :], in0=hn[s][:, :], in1=sh[:, :], op=ALU.add),
                 reads=[b_hn[s], bs], writes=[b_hf[s]])
            k.op("act", lambda e, s=s: e.copy(out=hb[s][:, :], in_=hf[s][:, :]), reads=[b_hf[s]], writes=[b_hb[s]])
            k.dma("sp", h2[i * 128:(i + 1) * 128, :], hb[s][:, :], reads=[b_hb[s]], writes=[h2b])
            for q4 in range(4):
                bank = q4
                for q in range(4):
                    kt = q4 * 4 + q
                    k.op("pe", lambda e, bank=bank, q=q, kt=kt, s=s: e.transpose(out=self.ps[bank][:, q * 128:(q + 1) * 128],
                                                                              in_=hf[s][:, kt * 128:(kt + 1) * 128], identity=idf[:, :]),
                         reads=[b_hf[s], b_idf], writes=[self.psb[bank]], sig=(q == 3))
                dst = hfT[s][:, q4 * 4:(q4 + 1) * 4, :]
                src = self.ps[bank][:, :].rearrange("p (q n) -> p q n", q=4)
                if q4 % 2 == 0:
                    k.op("dve", lambda e, dst=dst, src=src: e.tensor_copy(out=dst, in_=src), reads=[self.psb[bank]], writes=[b_hfT[s]])
                else:
                    k.op("act", lambda e, dst=dst, src=src: e.copy(out=dst, in_=src), reads=[self.psb[bank]], writes=[b_hfT[s]])
            lb = 4 + s
            for kt in range(16):
                k.op("pe", lambda e, lb=lb, kt=kt, s=s: e.matmul(self.ps[lb][:, 0:NE], lhsT=hfT[s][:, kt, :], rhs=wr[:, kt, :],
                                                             start=(kt == 0), stop=(kt == 15)),
                     reads=[b_hfT[s], b_wr], writes=[self.psb[lb]], sig=(kt == 15))
            mx, nmx, se, rse = sm[s][:, 2:3], sm[s][:, 3:4], sm[s][:, 4:5], sm[s][:, 5:6]
            k.op("dve", lambda e, lb=lb, mx=mx: e.reduce_max(out=mx, in_=self.ps[lb][:, 0:NE], axis=AX.X),
                 reads=[self.psb[lb]], writes=[b_mx[s]])
            k.op("dve", lambda e, mx=mx, nmx=nmx: e.tensor_scalar(out=nmx, in0=mx, scalar1=-1.0, scalar2=None, op0=ALU.mult),
                 reads=[b_mx[s]], writes=[b_nmx[s]])
            k.op("act", lambda e, lb=lb, s=s, nmx=nmx, se=se: e.activation(out=ex[s][:, :], in_=self.ps[lb][:, 0:NE], func=AF.Exp,
                                                                        bias=nmx, scale=1.0, accum_out=se),
                 reads=[self.psb[lb], b_nmx[s]], writes=[b_ex[s], b_se[s]])
            k.op("dve", lambda e, se=se, rse=rse: e.reciprocal(out=rse, in_=se), reads=[b_se[s]], writes=[b_rse[s]])
            k.op("dve", lambda e, s=s, i=i, rse=rse: e.tensor_scalar(out=aff_all[:, i, :], in0=ex[s][:, :], scalar1=rse, scalar2=None,
                                                                op0=ALU.mult),
                 reads=[b_ex[s], b_rse[s]], writes=[b_aff])
        if "aff_dbg" in self.debug_outs:
            ad = self.scratch("aff_dbg", [128, NT, NE], F32)
            k.dma("sp", ad.ap(), aff_all[:, :, :], reads=[b_aff], writes=[self.dbuf["aff_dbg"]])
    k.barrier()


def phase_route(self, l):
    k, nc = self.k, self.nc
    self.load_const("tri")
    self.load_const("tid")
    self.load_const("oob")
    tri, b_tri = self.c["tri"], self.cb["tri"]
    tid, b_tid = self.c["tid"], self.cb["tid"]
    if not hasattr(self, "b_idx"):
        self.idxl = [self.scratch("idxl%d" % e, [CAP + 256, 2], I32) for e in range(NE)]
        self.idxc = [self.scratch("idxc%d" % e, [128, 2], I32) for e in range(NE)]
        self.b_idx = [k.buf("idx%d" % e) for e in range(NE)]
        oob = self.c["oob"]
        for e in range(NE):
            k.dma("sp", self.idxl[e].ap().rearrange("(p a) t -> p (a t)", p=128), oob[:, 0:(CAP + 256) * 2 // 128],
                  reads=[self.cb["oob"]], writes=[self.b_idx[e]])
            k.dma("sp", self.idxc[e].ap().rearrange("(p a) t -> p (a t)", p=128), oob[:, 0:2], reads=[self.cb["oob"]], writes=[self.b_idx[e]])
    aff_all, b_aff = self.aff_all, self.b_aff
    if not hasattr(self, "rt_pos"):
        self.rt_pos = self.sb(self.st, "rt_pos", [128, NE, NT], I32)
        self.rt_pairs = self.sb(self.st, "rt_pairs", [128, NE, NT, 2], I32)
        self.b_pos, self.b_pairs = k.buf("rt_pos"), k.buf("rt_pairs")
    pos_i, pairs, b_pos, b_pairs = self.rt_pos, self.rt_pairs, self.b_pos, self.b_pairs
    with ExitStack() as st:
        Ac = self.sb(st, "rt_Ac", [128, NE, NT], F32)
        cmp_ = self.sb(st, "rt_cmp", [128, NE, NT], F32)
        tf = self.sb(st, "rt_tf", [128, NE, NT], F32)
        lo = self.sb(st, "rt_lo", [128, 2 * NE], F32)
        mid = self.sb(st, "rt_mid", [128, 2 * NE], F32)
        red = self.sb(st, "rt_red", [128, 2 * NE], F32)
        ge = self.sb(st, "rt_ge", [128, 2 * NE], F32)
        off = self.sb(st, "rt_off", [128, 2 * NE], F32)
        b_Ac, b_cmp, b_tf, b_lo, b_mid, b_red, b_ge, b_off = (k.buf() for _ in range(8))
        k.op("dve", lambda e: e.tensor_copy(out=Ac[:, :, :], in_=aff_all[:, :, :].rearrange("p i e -> p e i")),
             reads=[b_aff], writes=[b_Ac])
        k.op("dve", lambda e: e.memset(lo[:, :], 0.0), writes=[b_lo])
        pb = self.psb[0]
        tot = self.ps[0][:, 0:2 * NE]
        NIT = 28
        for it in range(NIT):
            ci = 2.0 ** (-(it + 1))
            k.op("dve", lambda e, ci=ci: e.tensor_scalar(out=mid[:, :], in0=lo[:, :], scalar1=ci, scalar2=None, op0=ALU.add),
                 reads=[b_lo], writes=[b_mid])
            k.op("dve", lambda e: e.tensor_tensor(out=cmp_[:, :, 0:NLT], in0=Ac[:, :, 0:NLT],
                                                  in1=mid[:, 0:NE].unsqueeze(2).to_broadcast([128, NE, NLT]), op=ALU.is_ge),
                 reads=[b_Ac, b_mid], writes=[b_cmp])
            k.op("dve", lambda e: e.tensor_tensor(out=cmp_[:, :, NLT:NT], in0=Ac[:, :, NLT:NT],
                                                  in1=mid[:, NE:2 * NE].unsqueeze(2).to_broadcast([128, NE, NT - NLT]), op=ALU.is_ge),
                 reads=[b_Ac, b_mid], writes=[b_cmp])
            k.op("dve", lambda e: e.reduce_sum(out=red[:, 0:NE], in_=cmp_[:, :, 0:NLT], axis=AX.X), reads=[b_cmp], writes=[b_red])
            k.op("dve", lambda e: e.reduce_sum(out=red[:, NE:2 * NE], in_=cmp_[:, :, NLT:NT], axis=AX.X), reads=[b_cmp], writes=[b_red])
            k.op("pe", lambda e: e.matmul(tot, lhsT=tri[:, 4, :], rhs=red[:, :], start=True, stop=True),
                 reads=[b_red, b_tri], writes=[pb])
            k.op("dve", lambda e, ci=ci: e.tensor_scalar(out=ge[:, 0:NE], in0=self.ps[0][:, 0:NE], scalar1=CAP - 0.5, scalar2=ci,
                                                       op0=ALU.is_ge, op1=ALU.mult), reads=[pb], writes=[b_ge])
            k.op("dve", lambda e, ci=ci: e.tensor_scalar(out=ge[:, NE:2 * NE], in0=self.ps[0][:, NE:2 * NE], scalar1=CAPC - 0.5, scalar2=ci,
                                                       op0=ALU.is_ge, op1=ALU.mult), reads=[pb], writes=[b_ge])
            k.op("dve", lambda e: e.tensor_tensor(out=lo[:, :], in0=lo[:, :], in1=ge[:, :], op=ALU.add), reads=[b_lo, b_ge], writes=[b_lo])
        k.op("dve", lambda e: e.tensor_tensor(out=cmp_[:, :, 0:NLT], in0=Ac[:, :, 0:NLT],
                                              in1=lo[:, 0:NE].unsqueeze(2).to_broadcast([128, NE, NLT]), op=ALU.is_ge),
             reads=[b_Ac, b_lo], writes=[b_cmp])
        k.op("dve", lambda e: e.tensor_tensor(out=cmp_[:, :, NLT:NT], in0=Ac[:, :, NLT:NT],
                                              in1=lo[:, NE:2 * NE].unsqueeze(2).to_broadcast([128, NE, NT - NLT]), op=ALU.is_ge),
             reads=[b_Ac, b_lo], writes=[b_cmp])
        k.op("dve", lambda e: e.reduce_sum(out=red[:, 0:NE], in_=cmp_[:, :, 0:NLT], axis=AX.X), reads=[b_cmp], writes=[b_red])
        k.op("dve", lambda e: e.reduce_sum(out=red[:, NE:2 * NE], in_=cmp_[:, :, NLT:NT], axis=AX.X), reads=[b_cmp], writes=[b_red])
        k.op("pe", lambda e: e.matmul(tot, lhsT=tri[:, 3, :], rhs=red[:, :], start=True, stop=True), reads=[b_red, b_tri], writes=[pb])
        k.op("dve", lambda e: e.tensor_copy(out=off[:, :], in_=tot), reads=[pb], writes=[b_off])
        ones_row = tri[:, 4, :]
        for e_ in range(NE):
            k.op("dve", lambda e, e_=e_: e.tensor_tensor_scan(out=tf[:, e_, 0:NLT], data0=ones_row[:, 0:NLT], data1=cmp_[:, e_, 0:NLT],
                                                             initial=off[:, e_:e_ + 1], op0=ALU.mult, op1=ALU.add),
                 reads=[b_cmp, b_off, b_tri], writes=[b_tf])
            k.op("dve", lambda e, e_=e_: e.tensor_tensor_scan(out=tf[:, e_, NLT:NT], data0=ones_row[:, 0:NT - NLT], data1=cmp_[:, e_, NLT:NT],
                                                             initial=off[:, NE + e_:NE + e_ + 1], op0=ALU.mult, op1=ALU.add),
                 reads=[b_cmp, b_off, b_tri], writes=[b_tf])
        k.op("dve", lambda e: e.tensor_scalar(out=tf[:, :, :], in0=tf[:, :, :], scalar1=-(1.0 + BIG), scalar2=None, op0=ALU.add),
             reads=[b_tf], writes=[b_tf])
        k.op("dve", lambda e: e.tensor_tensor(out=tf[:, :, :], in0=tf[:, :, :], in1=cmp_[:, :, :], op=ALU.mult),
             reads=[b_tf, b_cmp], writes=[b_tf])
        k.op("dve", lambda e: e.tensor_scalar(out=tf[:, :, :], in0=tf[:, :, :], scalar1=BIG, scalar2=None, op0=ALU.add),
             reads=[b_tf], writes=[b_tf])
        k.op("dve", lambda e: e.tensor_copy(out=pos_i[:, :, :], in_=tf[:, :, :]), reads=[b_tf], writes=[b_pos])
        k.op("dve", lambda e: e.tensor_copy(out=pairs[:, :, :, 0], in_=tid[:, :].unsqueeze(1).to_broadcast([128, NE, NT])),
             reads=[b_tid], writes=[b_pairs])
        pf = pairs.ap().bitcast(F32)
        k.op("dve", lambda e: e.tensor_copy(out=pf[:, :, :, 1], in_=Ac[:, :, :]), reads=[b_Ac], writes=[b_pairs])
        if "pos_dbg" in self.debug_outs:
            pd = self.scratch("pos_dbg", [128, NE, NT], I32)
            k.dma("sp", pd.ap(), pos_i[:, :, :], reads=[b_pos], writes=[self.dbuf["pos_dbg"]])
            td = self.scratch("thr_dbg", [128, 2 * NE], F32)
            k.dma("sp", td.ap(), lo[:, :], reads=[b_lo], writes=[self.dbuf["thr_dbg"]])
    k.barrier()


def route_scatter(self, e_):
    k = self.k
    idxl, idxc = self.idxl, self.idxc
    pos_i, pairs = self.rt_pos, self.rt_pairs
    for i in range(NT):
        if i < NLT:
            dst, bc = idxl[e_][:, :], CAP - 1
        else:
            dst, bc = idxc[e_][:, :], CAPC - 1
        k.idma(dst, bass.IndirectOffsetOnAxis(ap=pos_i[:, e_, i:i + 1], axis=0), pairs[:, e_, i, :], None,
               reads=[self.b_pos, self.b_pairs], writes=[self.b_idx[e_]], bounds_check=bc, oob_is_err=False)


Prog.phase_mod_moe = phase_mod_moe
Prog.phase_route = phase_route
Prog.route_scatter = route_scatter


def phase_experts(self, l):
    k, nc = self.k, self.nc
    h2, h2b = self.dram["h2"], self.dbuf["h2"]
    mo, mob = self.dram["moe_out"], self.dbuf["moe_out"]
    idxl, idxc = self.idxl, self.idxc
    self.load_const("ident_b")
    idb, b_id = self.c["ident_b"], self.cb["ident_b"]
    wg_d = k.inp("moe_w_gate_%d" % l, [NE, D, FF])
    wu_d = k.inp("moe_w_up_%d" % l, [NE, D, FF])
    wd_d = k.inp("moe_w_down_%d" % l, [NE, FF, D])
    NST = 17
    with ExitStack() as st:
        wg = self.sb(st, "ex_wg", [128, 16, FF], BF16)
        wu = self.sb(st, "ex_wu", [128, 16, FF], BF16)
        wd = self.sb(st, "ex_wd", [128, 8, D], BF16)
        stg = [self.sb(st, "ex_stg%d" % i, [128, 2048], F32) for i in range(2)]
        it = [self.sb(st, "ex_it%d" % i, [128, NST, 2], I32) for i in range(2)]
        xg = [self.sb(st, "ex_xg%d" % i, [128, D], BF16) for i in range(2)]
        xsT = self.sb(st, "ex_xsT", [128, 16, 512], BF16)
        sa = [self.sb(st, "ex_sa%d" % i, [128, 512], F32) for i in range(2)]
        hm = self.sb(st, "ex_hm", [128, 8, 512], BF16)
        ysb = [self.sb(st, "ex_y0", [128, D], F32)] * 2
        b_wg, b_wu, b_wd = k.buf(), k.buf(), k.buf()
        b_stg = [k.buf(), k.buf()]
        b_it = [k.buf(), k.buf()]
        b_xg = [k.buf(), k.buf()]
        b_xsT, b_hm = k.buf(), k.buf()
        b_sa = [k.buf(), k.buf()]
        b_y = [k.buf()] * 2
        for s in range(2):
            k.op("pool", lambda e, s=s: e.memset(xg[s][:, :], 0.0), writes=[b_xg[s]])
        self.route_scatter(0)
        sidx = 0
        xi = 0
        yi = 0
        last_scatter = None
        for e_ in range(NE):
            if e_ + 1 < NE:
                self.route_scatter(e_ + 1)
            wgv = wg_d[e_].rearrange("(kt p) f -> p kt f", p=128)
            wuv = wu_d[e_].rearrange("(kt p) f -> p kt f", p=128)
            wdv = wd_d[e_].rearrange("(ft p) d -> p ft d", p=128)
            for (dst, bdst, srcv, nchunk, per) in ((wg, b_wg, wgv, 8, 2), (wu, b_wu, wuv, 8, 2), (wd, b_wd, wdv, 8, 1)):
                for c in range(nchunk):
                    s = sidx % 2
                    sidx += 1
                    w = dst.shape[2]
                    sv = stg[s][:, :].rearrange("p (a f) -> p a f", a=per)
                    k.dma("sp", sv, srcv[:, c * per:(c + 1) * per, :], writes=[b_stg[s]])
                    k.op("act", lambda e, dst=dst, c=c, per=per, sv=sv: e.copy(out=dst[:, c * per:(c + 1) * per, :], in_=sv),
                         reads=[b_stg[s]], writes=[bdst])
            ip = e_ % 2
            with nc.allow_non_contiguous_dma(reason="8B idx pairs"):
                k.dma("sp", it[ip][:, 0:16, :], idxl[e_][0:CAP, :].rearrange("(st p) t -> p st t", p=128),
                      reads=[self.b_idx[e_]], writes=[b_it[ip]])
                k.dma("sp", it[ip][:, 16, :], idxc[e_][:, :], reads=[self.b_idx[e_]], writes=[b_it[ip]])
            itf = it[ip].ap().bitcast(F32)
            groups = [(0, 4), (4, 4), (8, 4), (12, 4), (16, 1)]
            for (st0, nst) in groups:
                nsl = nst * 128
                for j in range(nst):
                    stile = st0 + j
                    xs_ = xi % 2
                    xi += 1
                    k.idma(xg[xs_][:, :], None, h2[:, :], bass.IndirectOffsetOnAxis(ap=it[ip][:, stile, 0:1], axis=0),
                           reads=[h2b, b_it[ip]], writes=[b_xg[xs_]], bounds_check=NTOK - 1, oob_is_err=False)
                    for half in range(2):
                        bank = half
                        pv = self.ps[bank][:, :].bitcast(BF16)
                        for q in range(8):
                            kt = half * 8 + q
                            k.op("pe", lambda e, pv=pv, q=q, kt=kt, xs_=xs_: e.transpose(out=pv[:, q * 128:(q + 1) * 128],
                                                                                     in_=xg[xs_][:, kt * 128:(kt + 1) * 128], identity=idb[:, :]),
                                 reads=[b_xg[xs_], b_id], writes=[self.psb[bank]], sig=(q == 7))
                        dst = xsT[:, half * 8:(half + 1) * 8, j * 128:(j + 1) * 128]
                        src = pv.rearrange("p (q n) -> p q n", q=8)
                        if half == 0:
                            k.op("dve", lambda e, dst=dst, src=src: e.tensor_copy(out=dst, in_=src), reads=[self.psb[bank]], writes=[b_xsT])
                        else:
                            k.op("act", lambda e, dst=dst, src=src: e.copy(out=dst, in_=src), reads=[self.psb[bank]], writes=[b_xsT])
                for ft in range(8):
                    ba, bu = 2 + ft % 2, 4 + ft % 2
                    for kt in range(16):
                        k.op("pe", lambda e, ba=ba, kt=kt, ft=ft, nsl=nsl: e.matmul(self.ps[ba][:, 0:nsl], lhsT=wg[:, kt, ft * 128:(ft + 1) * 128],
                                                                                 rhs=xsT[:, kt, 0:nsl], start=(kt == 0), stop=(kt == 15)),
                             reads=[b_wg, b_xsT], writes=[self.psb[ba]], sig=(kt == 15))
                    for kt in range(16):
                        k.op("pe", lambda e, bu=bu, kt=kt, ft=ft, nsl=nsl: e.matmul(self.ps[bu][:, 0:nsl], lhsT=wu[:, kt, ft * 128:(ft + 1) * 128],
                                                                                 rhs=xsT[:, kt, 0:nsl], start=(kt == 0), stop=(kt == 15)),
                             reads=[b_wu, b_xsT], writes=[self.psb[bu]], sig=(kt == 15))
                    sp_ = ft % 2
                    k.op("act", lambda e, ba=ba, sp_=sp_, nsl=nsl: e.activation(out=sa[sp_][:, 0:nsl], in_=self.ps[ba][:, 0:nsl], func=AF.Silu),
                         reads=[self.psb[ba]], writes=[b_sa[sp_]])
                    k.op("dve", lambda e, bu=bu, sp_=sp_, ft=ft, nsl=nsl: e.tensor_tensor(out=hm[:, ft, 0:nsl], in0=sa[sp_][:, 0:nsl],
                                                                                         in1=self.ps[bu][:, 0:nsl], op=ALU.mult),
                         reads=[b_sa[sp_], self.psb[bu]], writes=[b_hm])
                for j in range(nst):
                    stile = st0 + j
                    ys = yi % 2
                    yi += 1
                    for dc in range(4):
                        by = 6 + dc % 2
                        for ft in range(8):
                            k.op("pe", lambda e, by=by, ft=ft, j=j, dc=dc: e.matmul(self.ps[by][:, :], lhsT=hm[:, ft, j * 128:(j + 1) * 128],
                                                                               rhs=wd[:, ft, dc * 512:(dc + 1) * 512], start=(ft == 0), stop=(ft == 7)),
                                 reads=[b_hm, b_wd], writes=[self.psb[by]], sig=(ft == 7))
                        gsc = itf[:, stile, 1:2]
                        if dc % 2 == 0:
                            k.op("dve", lambda e, by=by, ys=ys, dc=dc, gsc=gsc: e.tensor_scalar(out=ysb[ys][:, dc * 512:(dc + 1) * 512], in0=self.ps[by][:, :],
                                                                                             scalar1=gsc, scalar2=None, op0=ALU.mult),
                                 reads=[self.psb[by], b_it[ip]], writes=[b_y[ys]])
                        else:
                            k.op("act", lambda e, by=by, ys=ys, dc=dc, gsc=gsc: e.activation(out=ysb[ys][:, dc * 512:(dc + 1) * 512], in_=self.ps[by][:, :],
                                                                                          func=AF.Copy, scale=gsc),
                                 reads=[self.psb[by], b_it[ip]], writes=[b_y[ys]])
                    if j == 0 and st0 == 0 and last_scatter is not None:
                        k.wait("pool", last_scatter)
                    last = k.idma(mo[:, :], bass.IndirectOffsetOnAxis(ap=it[ip][:, stile, 0:1], axis=0), ysb[ys][:, :], None,
                                  reads=[b_y[ys], b_it[ip]], writes=[mob], bounds_check=NTOK - 1, oob_is_err=False, compute_op=ALU.add)
            last_scatter = last
    k.barrier()


def phase_combine(self, l):
    k, nc = self.k, self.nc
    xs, xb = self.dram["xs"], self.dbuf["xs"]
    mo, mob = self.dram["moe_out"], self.dbuf["moe_out"]
    with ExitStack() as st:
        gt = self.load_gate(st, l, 2, "cb_")
        xt = [self.sb(st, "cb_x%d" % i, [128, D], F32) for i in range(2)]
        mt = [self.sb(st, "cb_m%d" % i, [128, D], F32) for i in range(2)]
        b_x, b_m = [k.buf(), k.buf()], [k.buf(), k.buf()]
        for i in range(NT):
            s = i % 2
            g, bg = gt[0 if i < NLT else 1]
            k.dma("sp", xt[s][:, :], xs[i * 128:(i + 1) * 128, :], reads=[xb], writes=[b_x[s]])
            k.dma("sp", mt[s][:, :], mo[i * 128:(i + 1) * 128, :], reads=[mob], writes=[b_m[s]])
            k.op("pool", lambda e, s=s, g=g: e.tensor_tensor(out=mt[s][:, :], in0=mt[s][:, :], in1=g[:, :], op=ALU.mult),
                 reads=[b_m[s], bg], writes=[b_m[s]])
            k.op("dve", lambda e, s=s: e.tensor_tensor(out=xt[s][:, :], in0=xt[s][:, :], in1=mt[s][:, :], op=ALU.add),
                 reads=[b_x[s], b_m[s]], writes=[b_x[s]])
            k.dma("sp", xs[i * 128:(i + 1) * 128, :], xt[s][:, :], reads=[b_x[s]], writes=[xb])
    k.barrier()


Prog.phase_experts = phase_experts
Prog.phase_combine = phase_combine


def phase_fnet(self, l):
    k, nc = self.k, self.nc
    hT, hTb = self.dram["hT"], self.dbuf["hT"]
    mixT = self.scratch("mixT", [DIN, NTOK], BF16)
    mxb = self.dbuf["mixT"]
    hTv = hT.ap().rearrange("(g t p) n -> g p t n", t=2, p=128)
    mxv = mixT.ap()
    with ExitStack() as st:
        for nm in ("f1", "f2", "cs256", "ps256", "tw"):
            self.load_const(nm, st)
        f1, f2, cs256, ps256, tw = (self.c[n] for n in ("f1", "f2", "cs256", "ps256", "tw"))
        cbs = [self.cb[n] for n in ("f1", "f2", "cs256", "ps256", "tw")]
        b_f1, b_f2, b_cs, b_ps, b_tw = cbs
        XT = self.sb(st, "fn_XT", [128, 2, SEQ], BF16)
        XC = self.sb(st, "fn_XC", [128, 2, CTX], BF16)
        Z = self.sb(st, "fn_Z", [128, 128, 128], BF16)
        TP = self.sb(st, "fn_TP", [128, 64, 256], BF16)
        A_ = [self.sb(st, "fn_A%d" % i, [128, 512], F32) for i in range(2)]
        B_ = [self.sb(st, "fn_B%d" % i, [128, 512], F32) for i in range(2)]
        ZC = self.sb(st, "fn_ZC", [128, 2, 512], BF16)
        FC = self.sb(st, "fn_FC", [128, 2, CTX], BF16)
        b_XT, b_XC, b_Z, b_TP, b_ZC, b_FC = (k.buf() for _ in range(6))
        b_A, b_B = [k.buf(), k.buf()], [k.buf(), k.buf()]
        fT = Z.ap().rearrange("p a b -> p (a b)")
        XTs = XT.ap().rearrange("p t (n1 n2) -> p t n2 n1", n2=128)
        bi = 0
        for g in range(NG):
            for hh in range(4):
                k.dma("sp", XT[:, :, hh * 4096:(hh + 1) * 4096], hTv[g, :, :, hh * 4096:(hh + 1) * 4096], reads=[hTb], writes=[b_XT])
            k.dma("sp", XC[:, :, :], hTv[g, :, :, SEQ:NTOK], reads=[hTb], writes=[b_XC])
            for qd in range(4):
                c0 = qd * 64
                for nb in range(32):
                    bank = bi % 8
                    bi += 1
                    for q in range(4):
                        n2 = nb * 4 + q
                        for t in range(2):
                            k.op("pe", lambda e, bank=bank, q=q, t=t, n2=n2, c0=c0: e.matmul(
                                self.ps[bank][:, q * 128:(q + 1) * 128], lhsT=XTs[:, t, n2, :], rhs=cs256[:, t, :, c0:c0 + 64],
                                start=(t == 0), stop=(t == 1)), reads=[b_XT, b_cs], writes=[self.psb[bank]], sig=(q == 3 and t == 1))
                    dst = Z[:, nb * 4:(nb + 1) * 4, :]
                    src = self.ps[bank][:, :].rearrange("p (q c) -> p q c", q=4)
                    if nb % 2 == 0:
                        k.op("act", lambda e, dst=dst, src=src: e.copy(out=dst, in_=src), reads=[self.psb[bank]], writes=[b_Z])
                    else:
                        k.op("dve", lambda e, dst=dst, src=src: e.tensor_copy(out=dst, in_=src), reads=[self.psb[bank]], writes=[b_Z])
                for cp in range(32):
                    bank = bi % 8
                    bi += 1
                    for q in range(2):
                        cc = cp * 2 + q
                        k.op("pe", lambda e, bank=bank, q=q, cc=cc: e.matmul(self.ps[bank][:, q * 256:(q + 1) * 256], lhsT=Z[:, :, cc],
                                                                         rhs=f1[:, 0, :], start=True, stop=False),
                             reads=[b_Z, b_f1], writes=[self.psb[bank]], sig=False)
                        k.op("pe", lambda e, bank=bank, q=q, cc=cc: e.matmul(self.ps[bank][:, q * 256:(q + 1) * 256], lhsT=Z[:, :, 64 + cc],
                                                                         rhs=f1[:, 1, :], start=False, stop=True),
                             reads=[b_Z, b_f1], writes=[self.psb[bank]], sig=(q == 1))
                    s = cp % 2
                    P4 = self.ps[bank][:, :].rearrange("p (c r k) -> p c r k", c=2, r=2)
                    WA = tw[:, 0, :, :].unsqueeze(1).to_broadcast([128, 2, 2, 128])
                    WB = tw[:, 1, :, :].unsqueeze(1).to_broadcast([128, 2, 2, 128])
                    A4 = A_[s][:, :].rearrange("p (c r k) -> p c r k", c=2, r=2)
                    B4 = B_[s][:, :].rearrange("p (c r k) -> p c r k", c=2, r=2)
                    k.op("dve", lambda e, A4=A4, P4=P4, WA=WA: e.tensor_tensor(out=A4, in0=P4, in1=WA, op=ALU.mult),
                         reads=[self.psb[bank], b_tw], writes=[b_A[s]])
                    k.op("dve", lambda e, B4=B4, P4=P4, WB=WB: e.tensor_tensor(out=B4, in0=P4, in1=WB, op=ALU.mult),
                         reads=[self.psb[bank], b_tw], writes=[b_B[s]])
                    k.op("pool", lambda e, A4=A4, cp=cp: e.tensor_tensor(out=TP[:, cp * 2:cp * 2 + 2, 0:128], in0=A4[:, :, 0, :], in1=A4[:, :, 1, :],
                                                                        op=ALU.subtract), reads=[b_A[s]], writes=[b_TP])
                    k.op("pool", lambda e, B4=B4, cp=cp: e.tensor_tensor(out=TP[:, cp * 2:cp * 2 + 2, 128:256], in0=B4[:, :, 0, :], in1=B4[:, :, 1, :],
                                                                        op=ALU.add), reads=[b_B[s]], writes=[b_TP])
                fTv = fT[0:64, :].rearrange("p (k2 k1) -> p k2 k1", k1=128)
                for kb in range(32):
                    bank = bi % 8
                    bi += 1
                    for q in range(4):
                        k1 = kb * 4 + q
                        k.op("pe", lambda e, bank=bank, q=q, k1=k1: e.matmul(self.ps[bank][0:64, q * 128:(q + 1) * 128], lhsT=TP[:, :, k1],
                                                                         rhs=f2[:, 0, :], start=True, stop=False),
                             reads=[b_TP, b_f2], writes=[self.psb[bank]], sig=False)
                        k.op("pe", lambda e, bank=bank, q=q, k1=k1: e.matmul(self.ps[bank][0:64, q * 128:(q + 1) * 128], lhsT=TP[:, :, 128 + k1],
                                                                         rhs=f2[:, 1, :], start=False, stop=True),
                             reads=[b_TP, b_f2], writes=[self.psb[bank]], sig=(q == 3))
                    dst = fTv[:, :, kb * 4:(kb + 1) * 4]
                    src = self.ps[bank][0:64, :].rearrange("p (a b) -> p b a", a=4)
                    if kb % 2 == 0:
                        k.op("act", lambda e, dst=dst, src=src: e.activation(out=dst, in_=src, func=AF.Copy, scale=1.0 / 2048.0),
                             reads=[self.psb[bank]], writes=[b_Z])
                    else:
                        k.op("dve", lambda e, dst=dst, src=src: e.tensor_scalar(out=dst, in0=src, scalar1=1.0 / 2048.0, scalar2=None, op0=ALU.mult),
                             reads=[self.psb[bank]], writes=[b_Z])
                r0 = g * 256 + c0
                for hh in range(2):
                    k.dma("sp", mxv[r0:r0 + 64, hh * 8192:(hh + 1) * 8192], fT[0:64, hh * 8192:(hh + 1) * 8192], reads=[b_Z], writes=[mxb])
            for nt in range(2):
                bank = bi % 8
                bi += 1
                for t in range(2):
                    k.op("pe", lambda e, bank=bank, t=t, nt=nt: e.matmul(self.ps[bank][:, :], lhsT=XC[:, t, nt * 128:(nt + 1) * 128], rhs=cs256[:, t, :, :],
                                                                     start=(t == 0), stop=(t == 1)),
                         reads=[b_XC, b_cs], writes=[self.psb[bank]], sig=(t == 1))
                k.op("act", lambda e, bank=bank, nt=nt: e.copy(out=ZC[:, nt, :], in_=self.ps[bank][:, :]), reads=[self.psb[bank]], writes=[b_ZC])
            for ct in range(2):
                bank = bi % 8
                bi += 1
                n = 0
                for nt in range(2):
                    for ri in range(2):
                        k.op("pe", lambda e, bank=bank, nt=nt, ri=ri, ct=ct, n=n: e.matmul(
                            self.ps[bank][:, 0:256], lhsT=ZC[:, nt, ri * 256 + ct * 128:ri * 256 + (ct + 1) * 128], rhs=ps256[:, nt, ri, :],
                            start=(n == 0), stop=(n == 3)), reads=[b_ZC, b_ps], writes=[self.psb[bank]], sig=(n == 3))
                        n += 1
                k.op("act", lambda e, bank=bank, ct=ct: e.activation(out=FC[:, ct, :], in_=self.ps[bank][:, 0:256], func=AF.Copy, scale=1.0 / 256.0),
                     reads=[self.psb[bank]], writes=[b_FC])
            k.dma("sp", mxv[g * 256:(g + 1) * 256, SEQ:NTOK].rearrange("(t p) n -> p t n", p=128), FC[:, :, :], reads=[b_FC], writes=[mxb])
    k.barrier()


def phase_outproj(self, l, wname, KT):
    k, nc = self.k, self.nc
    xs, xb = self.dram["xs"], self.dbuf["xs"]
    mixT, mxb = self.dram["mixT"], self.dbuf["mixT"]
    w_d = k.inp(wname, [KT * 128, D])
    wv = w_d.ap().rearrange("(kt p) d -> p kt d", p=128)
    mv = mixT.ap()[0:KT * 128, :].rearrange("(kt p) n -> p kt n", p=128)
    with ExitStack() as st:
        gt = self.load_gate(st, l, 1, "op_")
        W = self.sb(st, "op_W", [128, KT, 1024], BF16)
        stg = [self.sb(st, "op_stg%d" % i, [128, 2, 1024], F32) for i in range(2)]
        mt = [self.sb(st, "op_m%d" % i, [128, KT, 128], BF16) for i in range(2)]
        xt = [self.sb(st, "op_x%d" % i, [128, 1024], F32) for i in range(2)]
        tmp = [self.sb(st, "op_t%d" % i, [128, 1024], F32) for i in range(2)]
        b_W = k.buf()
        b_stg, b_m, b_x, b_t = ([k.buf(), k.buf()] for _ in range(4))
        si = 0
        for ch in range(2):
            for c in range(KT // 2):
                s = si % 2
                si += 1
                k.dma("sp", stg[s][:, :, :], wv[:, c * 2:(c + 1) * 2, ch * 1024:(ch + 1) * 1024], writes=[b_stg[s]])
                k.op("act", lambda e, s=s, c=c: e.copy(out=W[:, c * 2:(c + 1) * 2, :], in_=stg[s][:, :, :]), reads=[b_stg[s]], writes=[b_W])
            for i in range(NT):
                s = i % 2
                g, bg = gt[0 if i < NLT else 1]
                with nc.allow_non_contiguous_dma(reason="256B runs"):
                    k.dma("sp", mt[s][:, :, :], mv[:, :, i * 128:(i + 1) * 128], reads=[mxb], writes=[b_m[s]])
                k.dma("sp", xt[s][:, :], xs[i * 128:(i + 1) * 128, ch * 1024:(ch + 1) * 1024], reads=[xb], writes=[b_x[s]])
                for cc in range(2):
                    bank = (i % 2) * 2 + cc
                    for kt in range(KT):
                        k.op("pe", lambda e, bank=bank, kt=kt, s=s, cc=cc: e.matmul(self.ps[bank][:, :], lhsT=mt[s][:, kt, :], rhs=W[:, kt, cc * 512:(cc + 1) * 512],
                                                                               start=(kt == 0), stop=(kt == KT - 1)),
                             reads=[b_m[s], b_W], writes=[self.psb[bank]], sig=(kt == KT - 1))
                    k.op("dve", lambda e, bank=bank, s=s, cc=cc, g=g, ch=ch: e.tensor_tensor(out=tmp[s][:, cc * 512:(cc + 1) * 512], in0=self.ps[bank][:, :],
                                                                                          in1=g[:, ch * 1024 + cc * 512:ch * 1024 + (cc + 1) * 512], op=ALU.mult),
                         reads=[self.psb[bank], bg], writes=[b_t[s]])
                k.op("pool", lambda e, s=s: e.tensor_tensor(out=xt[s][:, :], in0=xt[s][:, :], in1=tmp[s][:, :], op=ALU.add),
                     reads=[b_x[s], b_t[s]], writes=[b_x[s]])
                k.dma("sp", xs[i * 128:(i + 1) * 128, ch * 1024:(ch + 1) * 1024], xt[s][:, :], reads=[b_x[s]], writes=[xb])
    k.barrier()


Prog.phase_fnet = phase_fnet
Prog.phase_outproj = phase_outproj


GRID_W = 64
RB = 32


def phase_ssd(self, l, groups=range(NG)):
    k, nc = self.k, self.nc
    kk = l // 2
    hT, hTb = self.dram["hT"], self.dbuf["hT"]
    mixT = self.scratch("mixT", [DIN, NTOK], BF16)
    mxb = self.dbuf["mixT"]
    xbc = self.scratch("xbc", [768, NTOK], F32)
    zs = self.scratch("zs", [512, NTOK], BF16)
    xc = self.scratch("xc", [768, NTOK], BF16)
    yf = self.scratch("yf", [NTOK, 512], F32)
    b_xbc, b_zs, b_xc, b_yf = (self.dbuf[n] for n in ("xbc", "zs", "xc", "yf"))
    for nm in ("ident_b", "tri", "msk"):
        self.load_const(nm)
    idb, b_id = self.c["ident_b"], self.cb["ident_b"]
    tri, b_tri = self.c["tri"], self.cb["tri"]
    msk, b_msk = self.c["msk"], self.cb["msk"]
    w_in = k.inp("ssd_w_in_%d" % kk, [D, 10368])
    cw_d = k.inp("ssd_conv_w_%d" % kk, [9, 6144])
    cb_d = k.inp("ssd_conv_b_%d" % kk, [1, 6144])
    dtb_d = k.inp("ssd_dt_bias_%d" % kk, [1, 128])
    alog_d = k.inp("ssd_a_log_%d" % kk, [1, 128])
    dsk_d = k.inp("ssd_d_%d" % kk, [1, 64])
    nw_d = k.inp("ssd_norm_w_%d" % kk, [1, DIN])
    hTv = hT.ap().rearrange("(kt p) n -> p kt n", p=128)
    winv = w_in.ap().rearrange("(kt p) c -> p kt c", p=128)
    chunks = [(c * 512, 512) for c in range(32)] + [(SEQ, 256)]
    with ExitStack() as sg:
        DT = self.sb(sg, "sd_DT", [128, NT, 32], F32)
        b_DT = k.buf()
        prm = self.sb(sg, "sd_prm", [128, 64], F32)
        b_prm = k.buf()
        nwb = self.sb(sg, "sd_nwb", [128, 512], F32)
        b_nwb = k.buf()
        for g in groups:
            segs = [(0, g * 512, 512), (512, 4096 + g * 512, 512), (1024, 8192 + g * 128, 128), (1152, 9216 + g * 128, 128),
                    (1280, 10240 + g * 8, 8), (1288, 10304 + g * 8, 8)]
            k.dma("sp", prm[:, 0:8], bview(dtb_d[0:1, g * 8:g * 8 + 8]), writes=[b_prm])
            k.dma("sp", prm[:, 8:16], bview(dtb_d[0:1, 64 + g * 8:64 + g * 8 + 8]), writes=[b_prm])
            k.dma("sp", prm[:, 40:48], bview(alog_d[0:1, g * 8:g * 8 + 8]), writes=[b_prm])
            k.dma("sp", prm[:, 48:56], bview(alog_d[0:1, 64 + g * 8:64 + g * 8 + 8]), writes=[b_prm])
            k.dma("sp", prm[:, 32:40], bview(dsk_d[0:1, g * 8:g * 8 + 8]), writes=[b_prm])
            k.dma("sp", nwb[:, :], bview(nw_d[0:1, g * 512:(g + 1) * 512]), writes=[b_nwb])
            k.op("act", lambda e: e.activation(out=prm[:, 16:32], in_=prm[:, 40:56], func=AF.Exp), reads=[b_prm], writes=[b_prm])
            k.op("dve", lambda e: e.tensor_scalar(out=prm[:, 16:32], in0=prm[:, 16:32], scalar1=-1.0, scalar2=None, op0=ALU.mult),
                 reads=[b_prm], writes=[b_prm])
            with ExitStack() as st:
                Wg = self.sb(st, "sd_Wg", [128, 16, GC], BF16)
                stg = [self.sb(st, "sd_stg%d" % i, [128, 4, 512], F32) for i in range(2)]
                hc = [self.sb(st, "sd_hc%d" % i, [128, 16, 512], BF16) for i in range(2)]
                ob = [self.sb(st, "sd_ob%d" % i, [128, 512], F32) for i in range(2)]
                zb = [self.sb(st, "sd_zb%d" % i, [128, 512], BF16) for i in range(2)]
                dtr = [self.sb(st, "sd_dtr%d" % i, [128, 4, 16], F32) for i in range(2)]
                b_Wg = k.buf()
                b_stg, b_hc, b_ob, b_zb, b_dtr = ([k.buf(), k.buf()] for _ in range(5))
                si = 0
                for (dcol, scol, w) in segs:
                    if w == 512:
                        for c in range(4):
                            s = si % 2
                            si += 1
                            k.dma("sp", stg[s][:, :, :], winv[:, c * 4:(c + 1) * 4, scol:scol + 512], writes=[b_stg[s]])
                            k.op("act", lambda e, s=s, c=c, dcol=dcol: e.copy(out=Wg[:, c * 4:(c + 1) * 4, dcol:dcol + 512], in_=stg[s][:, :, :]),
                                 reads=[b_stg[s]], writes=[b_Wg])
                    else:
                        s = si % 2
                        si += 1
                        sv = stg[s].ap().rearrange("p a b -> p (a b)")[:, 0:16 * w].rearrange("p (a b) -> p a b", a=16)
                        with nc.allow_non_contiguous_dma(reason="narrow weight columns"):
                            k.dma("sp", sv, winv[:, :, scol:scol + w], writes=[b_stg[s]])
                        k.op("act", lambda e, sv=sv, dcol=dcol, w=w: e.copy(out=Wg[:, :, dcol:dcol + w], in_=sv), reads=[b_stg[s]], writes=[b_Wg])
                oi = 0
                for ci, (t0, nt) in enumerate(chunks):
                    s = ci % 2
                    k.dma("sp", hc[s][:, :, 0:nt], hTv[:, :, t0:t0 + nt], reads=[hTb], writes=[b_hc[s]])
                    for ct in range(10):
                        bank = ct % 4
                        for kt in range(16):
                            k.op("pe", lambda e, bank=bank, kt=kt, ct=ct, s=s, nt=nt: e.matmul(self.ps[bank][:, 0:nt], lhsT=Wg[:, kt, ct * 128:(ct + 1) * 128],
                                                                                          rhs=hc[s][:, kt, 0:nt], start=(kt == 0), stop=(kt == 15)),
                                 reads=[b_Wg, b_hc[s]], writes=[self.psb[bank]], sig=(kt == 15))
                        o = oi % 2
                        oi += 1
                        if ct < 4:
                            k.op("act", lambda e, bank=bank, o=o, nt=nt: e.activation(out=zb[o][:, 0:nt], in_=self.ps[bank][:, 0:nt], func=AF.Silu),
                                 reads=[self.psb[bank]], writes=[b_zb[o]])
                            k.dma("sp", zs[ct * 128:(ct + 1) * 128, t0:t0 + nt], zb[o][:, 0:nt], reads=[b_zb[o]], writes=[b_zs])
                        else:
                            k.op("dve", lambda e, bank=bank, o=o, nt=nt: e.tensor_copy(out=ob[o][:, 0:nt], in_=self.ps[bank][:, 0:nt]),
                                 reads=[self.psb[bank]], writes=[b_ob[o]])
                            k.dma("sp", xbc[(ct - 4) * 128:(ct - 3) * 128, t0:t0 + nt], ob[o][:, 0:nt], reads=[b_ob[o]], writes=[b_xbc])
                    ntile = nt // 128
                    bank = 4 + ci % 2
                    for j in range(ntile):
                        for kt in range(16):
                            k.op("pe", lambda e, bank=bank, kt=kt, j=j, s=s: e.matmul(self.ps[bank][:, j * 16:(j + 1) * 16], lhsT=hc[s][:, kt, j * 128:(j + 1) * 128],
                                                                                 rhs=Wg[:, kt, 1280:1296], start=(kt == 0), stop=(kt == 15)),
                                 reads=[b_Wg, b_hc[s]], writes=[self.psb[bank]], sig=(kt == 15 and j == ntile - 1))
                    d = ci % 2
                    dv = dtr[d][:, 0:ntile, :]
                    k.op("dve", lambda e, bank=bank, dv=dv, ntile=ntile: e.tensor_tensor(
                        out=dv, in0=self.ps[bank][:, 0:ntile * 16].rearrange("p (j c) -> p j c", c=16),
                        in1=prm[:, 0:16].unsqueeze(1).to_broadcast([128, ntile, 16]), op=ALU.add),
                         reads=[self.psb[bank], b_prm], writes=[b_dtr[d]])
                    k.op("act", lambda e, dv=dv: e.activation(out=dv, in_=dv, func=AF.Exp), reads=[b_dtr[d]], writes=[b_dtr[d]])
                    tl0 = t0 // 128
                    k.op("act", lambda e, dv=dv, tl0=tl0, ntile=ntile: e.activation(out=DT[:, tl0:tl0 + ntile, 0:16], in_=dv, func=AF.Ln, bias=1.0),
                         reads=[b_dtr[d]], writes=[b_DT])
                    k.op("dve", lambda e, tl0=tl0, ntile=ntile: e.tensor_tensor(out=DT[:, tl0:tl0 + ntile, 16:32], in0=DT[:, tl0:tl0 + ntile, 0:16],
                                                                             in1=prm[:, 16:32].unsqueeze(1).to_broadcast([128, ntile, 16]), op=ALU.mult),
                         reads=[b_DT, b_prm], writes=[b_DT])
            k.barrier()
            with ExitStack() as st:
                cw = self.sb(st, "sd_cw", [128, 6, 9], F32)
                cbias = self.sb(st, "sd_cb", [128, 6], F32)
                pin = [self.sb(st, "sd_pin%d" % i, [128, RB + 2, GRID_W + 2], F32) for i in range(2)]
                pinc = self.sb(st, "sd_pinc", [128, CTX + 2], F32)
                acc = [self.sb(st, "sd_acc%d" % i, [128, RB, GRID_W], F32) for i in range(2)]
                co = [self.sb(st, "sd_co%d" % i, [128, RB * GRID_W], BF16) for i in range(2)]
                b_cw = k.buf()
                b_pin, b_acc, b_co = ([k.buf(), k.buf()] for _ in range(3))
                b_pinc = k.buf()
                choff = [g * 512 + t * 128 for t in range(4)] + [4096 + g * 128, 5120 + g * 128]
                with nc.allow_non_contiguous_dma(reason="conv taps transposed"):
                    for ct in range(6):
                        k.dma("sp", cw[:, ct, :], cw_d[:, choff[ct]:choff[ct] + 128].rearrange("t c -> c t"), writes=[b_cw])
                        k.dma("sp", cbias[:, ct:ct + 1], cb_d[0:1, choff[ct]:choff[ct] + 128].rearrange("o c -> c o"), writes=[b_cw])
                for s in range(2):
                    k.op("pool", lambda e, s=s: e.memset(pin[s][:, :, :], 0.0), writes=[b_pin[s]])
                k.op("pool", lambda e: e.memset(pinc[:, :], 0.0), writes=[b_pinc])
                nblk = SEQ // (RB * GRID_W)
                bi = 0
                for ct in range(6):
                    for blk in range(nblk):
                        s = bi % 2
                        bi += 1
                        r0 = blk * RB
                        ra, rb_ = max(r0 - 1, 0), min(r0 + RB + 1, SEQ // GRID_W)
                        pa = ra - (r0 - 1)
                        if blk == 0:
                            k.op("pool", lambda e, s=s: e.memset(pin[s][:, 0, :], 0.0), writes=[b_pin[s]])
                        if blk == nblk - 1:
                            k.op("pool", lambda e, s=s: e.memset(pin[s][:, RB + 1, :], 0.0), writes=[b_pin[s]])
                        k.dma("sp", pin[s][:, pa:pa + (rb_ - ra), 1:GRID_W + 1],
                              xbc[ct * 128:(ct + 1) * 128, ra * GRID_W:rb_ * GRID_W].rearrange("p (r c) -> p r c", c=GRID_W),
                              reads=[b_xbc], writes=[b_pin[s]])
                        k.op("act", lambda e, s=s, ct=ct: e.activation(out=acc[s][:, :, :], in_=pin[s][:, 0:RB, 0:GRID_W], func=AF.Identity,
                                                                    scale=cw[:, ct, 0:1], bias=cbias[:, ct:ct + 1]),
                             reads=[b_pin[s], b_cw], writes=[b_acc[s]])
                        for tp in range(1, 9):
                            ky, kx = tp // 3, tp % 3
                            eng = "dve"
                            k.op(eng, lambda e, s=s, ct=ct, tp=tp, ky=ky, kx=kx: e.scalar_tensor_tensor(
                                out=acc[s][:, :, :], in0=pin[s][:, ky:ky + RB, kx:kx + GRID_W], scalar=cw[:, ct, tp:tp + 1], in1=acc[s][:, :, :],
                                op0=ALU.mult, op1=ALU.add), reads=[b_pin[s], b_cw, b_acc[s]], writes=[b_acc[s]])
                        k.op("act", lambda e, s=s: e.activation(out=co[s][:, :], in_=acc[s].ap().rearrange("p r c -> p (r c)"), func=AF.Silu),
                             reads=[b_acc[s]], writes=[b_co[s]])
                        k.dma("sp", xc[ct * 128:(ct + 1) * 128, r0 * GRID_W:(r0 + RB) * GRID_W], co[s][:, :], reads=[b_co[s]], writes=[b_xc])
                    s = bi % 2
                    bi += 1
                    k.dma("sp", pinc[:, 1:CTX + 1], xbc[ct * 128:(ct + 1) * 128, SEQ:NTOK], reads=[b_xbc], writes=[b_pinc])
                    av = acc[s].ap().rearrange("p r c -> p (r c)")[:, 0:CTX]
                    k.op("act", lambda e, av=av, ct=ct: e.activation(out=av, in_=pinc[:, 0:CTX], func=AF.Identity,
                                                                  scale=cw[:, ct, 3:4], bias=cbias[:, ct:ct + 1]),
                         reads=[b_pinc, b_cw], writes=[b_acc[s]])
                    for kx in (1, 2):
                        k.op("dve", lambda e, av=av, ct=ct, kx=kx: e.scalar_tensor_tensor(out=av, in0=pinc[:, kx:kx + CTX], scalar=cw[:, ct, 3 + kx:4 + kx], in1=av,
                                                                                       op0=ALU.mult, op1=ALU.add),
                             reads=[b_pinc, b_cw, b_acc[s]], writes=[b_acc[s]])
                    k.op("act", lambda e, s=s, av=av: e.activation(out=co[s][:, 0:CTX], in_=av, func=AF.Silu), reads=[b_acc[s]], writes=[b_co[s]])
                    k.dma("sp", xc[ct * 128:(ct + 1) * 128, SEQ:NTOK], co[s][:, 0:CTX], reads=[b_co[s]], writes=[b_xc])
            k.barrier()
            self.ssd_scan(g, DT, b_DT, prm, b_prm, nwb, b_nwb)
            k.barrier()


def ssd_scan(self, g, DT, b_DT, prm, b_prm, nwb, b_nwb):
    k, nc = self.k, self.nc
    mixT, mxb = self.dram["mixT"], self.dbuf["mixT"]
    zs, xc, yf = self.dram["zs"], self.dram["xc"], self.dram["yf"]
    b_zs, b_xc, b_yf = self.dbuf["zs"], self.dbuf["xc"], self.dbuf["yf"]
    idb, b_id = self.c["ident_b"], self.cb["ident_b"]
    tri, b_tri = self.c["tri"], self.cb["tri"]
    msk, b_msk = self.c["msk"], self.cb["msk"]
    xcv = xc.ap().rearrange("(t p) n -> p t n", p=128)
    zsv = zs.ap().rearrange("(t p) n -> p t n", p=128)
    mxv = mixT.ap()[g * 512:(g + 1) * 512, :].rearrange("(t p) n -> p t n", p=128)
    PS = self.ps
    PB = self.psb
    with ExitStack() as st:
        xcT = [self.sb(st, "sc_xcT%d" % i, [128, 6, 128], BF16) for i in range(2)]
        zT = [self.sb(st, "sc_zT%d" % i, [128, 4, 128], BF16) for i in range(2)]
        yfl = [self.sb(st, "sc_yfl%d" % i, [128, 512], F32) for i in range(2)]
        xdt = self.sb(st, "sc_xdt", [128, 8, 64], BF16)
        xD = self.sb(st, "sc_xD", [128, 8, 64], F32)
        Btok = self.sb(st, "sc_Btok", [128, 128], BF16)
        R = self.sb(st, "sc_R", [128, 8, 128], F32)
        E = self.sb(st, "sc_E", [128, 8, 128], F32)
        Mt = self.sb(st, "sc_Mt", [128, 8, 128], BF16)
        ex24 = self.sb(st, "sc_ex24", [128, 24], F32)
        CBm = self.sb(st, "sc_CBm", [128, 128], F32)
        t1 = self.sb(st, "sc_t1", [128, 8, 64], F32)
        t2 = [self.sb(st, "sc_t2%d" % i, [128, 8, 64], F32) for i in range(2)]
        xw = self.sb(st, "sc_xw", [128, 8, 64], BF16)
        ST = self.sb(st, "sc_ST", [128, 8, 64], F32)
        STb = self.sb(st, "sc_STb", [128, 8, 64], BF16)
        yz = self.sb(st, "sc_yz", [128, 512], F32)
        junk = self.sb(st, "sc_junk", [128, 512], BF16)
        yn = self.sb(st, "sc_yn", [128, 512], BF16)
        ynT = [self.sb(st, "sc_ynT%d" % i, [128, 4, 128], BF16) for i in range(2)]
        sm = self.sb(st, "sc_sm", [128, 4], F32)
        b_xcT, b_zT, b_yfl, b_t2, b_ynT = ([k.buf(), k.buf()] for _ in range(5))
        (b_xdt, b_xD, b_Btok, b_R, b_E, b_Mt, b_ex, b_CBm, b_t1, b_xw, b_ST, b_STb, b_yz, b_j, b_yn, b_ss, b_rs) = (k.buf() for _ in range(17))
        order_f = [128, 129] + list(range(128))
        order_b = [129, 128] + list(range(127, -1, -1))
        it = 0
        for dr, order in ((0, order_f), (1, order_b)):
            Tm, Um = (tri[:, 0, :], tri[:, 1, :]) if dr == 0 else (tri[:, 2, :], tri[:, 3, :])
            ones = tri[:, 4, :]
            mk = msk[:, dr, :]
            k.op("pool", lambda e: e.memset(ST[:, :, :], 0.0), writes=[b_ST])
            k.op("pool", lambda e: e.memset(STb[:, :, :], 0.0), writes=[b_STb])
            for ti in order:
                s = it % 2
                it += 1
                tok0 = ti * 128
                with nc.allow_non_contiguous_dma(reason="256B runs"):
                    k.dma("sp", xcT[s][:, :, :], xcv[:, :, tok0:tok0 + 128], reads=[b_xc], writes=[b_xcT[s]])
                    if dr == 1:
                        k.dma("sp", zT[s][:, :, :], zsv[:, :, tok0:tok0 + 128], reads=[b_zs], writes=[b_zT[s]])
                        k.dma("sp", yfl[s][:, :], yf[tok0:tok0 + 128, :], reads=[b_yf], writes=[b_yfl[s]])
                dtv = DT[:, ti, dr * 8:(dr + 1) * 8]
                dav = DT[:, ti, 16 + dr * 8:16 + (dr + 1) * 8]
                p0 = PS[0][:, :].bitcast(BF16)
                for q in range(5):
                    k.op("pe", lambda e, q=q, s=s: e.transpose(out=p0[:, q * 128:(q + 1) * 128], in_=xcT[s][:, q, :], identity=idb[:, :]),
                         reads=[b_xcT[s], b_id], writes=[PB[0]], sig=(q == 4))
                xv = p0[:, 0:512].rearrange("p (h d) -> p h d", h=8)
                k.op("dve", lambda e, xv=xv, dtv=dtv: e.tensor_tensor(out=xdt[:, :, :], in0=xv, in1=dtv.unsqueeze(2).to_broadcast([128, 8, 64]), op=ALU.mult),
                     reads=[PB[0], b_DT], writes=[b_xdt])
                if dr == 0:
                    k.op("dve", lambda e, xv=xv: e.tensor_tensor(out=xD[:, :, :], in0=xv, in1=prm[:, 32:40].unsqueeze(2).to_broadcast([128, 8, 64]), op=ALU.mult),
                         reads=[PB[0], b_prm], writes=[b_xD])
                k.op("act", lambda e: e.copy(out=Btok[:, :], in_=p0[:, 512:640]), reads=[PB[0]], writes=[b_Btok])
                k.op("pool", lambda e, Tm=Tm, dav=dav: e.tensor_tensor(out=R[:, :, :], in0=Tm.unsqueeze(1).to_broadcast([128, 8, 128]),
                                                                    in1=dav.unsqueeze(2).to_broadcast([128, 8, 128]), op=ALU.mult),
                     reads=[b_tri, b_DT], writes=[b_R])
                Rf = R.ap().rearrange("p h l -> p (h l)")
                for hb in range(2):
                    k.op("pe", lambda e, hb=hb, Um=Um, Rf=Rf: e.matmul(PS[1 + hb][:, :], lhsT=Um, rhs=Rf[:, hb * 512:(hb + 1) * 512], start=True, stop=True),
                         reads=[b_R, b_tri], writes=[PB[1 + hb]])
                k.op("pe", lambda e, Tm=Tm, dav=dav: e.matmul(PS[3][:, 128:136], lhsT=Tm, rhs=dav, start=True, stop=True), reads=[b_tri, b_DT], writes=[PB[3]], sig=False)
                k.op("pe", lambda e, Um=Um, dav=dav: e.matmul(PS[3][:, 136:144], lhsT=Um, rhs=dav, start=True, stop=True), reads=[b_tri, b_DT], writes=[PB[3]], sig=False)
                k.op("pe", lambda e, ones=ones, dav=dav: e.matmul(PS[3][:, 144:152], lhsT=ones, rhs=dav, start=True, stop=True), reads=[b_tri, b_DT], writes=[PB[3]], sig=False)
                k.op("pe", lambda e, s=s: e.matmul(PS[3][:, 0:128], lhsT=xcT[s][:, 4, :], rhs=xcT[s][:, 5, :], start=True, stop=True),
                     reads=[b_xcT[s]], writes=[PB[3]])
                k.op("act", lambda e: e.activation(out=ex24[:, :], in_=PS[3][:, 128:152], func=AF.Exp), reads=[PB[3]], writes=[b_ex])
                k.op("dve", lambda e, mk=mk: e.tensor_tensor(out=CBm[:, :], in0=PS[3][:, 0:128], in1=mk, op=ALU.mult), reads=[PB[3], b_msk], writes=[b_CBm])
                for hb in range(2):
                    k.op("act", lambda e, hb=hb: e.activation(out=E[:, hb * 4:(hb + 1) * 4, :], in_=PS[1 + hb][:, :].rearrange("p (h l) -> p h l", h=4), func=AF.Exp),
                         reads=[PB[1 + hb]], writes=[b_E])
                k.op("dve", lambda e: e.tensor_tensor(out=Mt[:, :, :], in0=E[:, :, :], in1=CBm[:, :].unsqueeze(1).to_broadcast([128, 8, 128]), op=ALU.mult),
                     reads=[b_E, b_CBm], writes=[b_Mt])
                for h in range(8):
                    k.op("pe", lambda e, h=h: e.matmul(PS[4][:, h * 64:(h + 1) * 64], lhsT=Mt[:, h, :], rhs=xdt[:, h, :], start=True, stop=True),
                         reads=[b_Mt, b_xdt], writes=[PB[4]], sig=(h == 7))
                STf = STb.ap().rearrange("p h d -> p (h d)")
                k.op("pe", lambda e, s=s, STf=STf: e.matmul(PS[5][:, :], lhsT=xcT[s][:, 5, :], rhs=STf, start=True, stop=True),
                     reads=[b_xcT[s], b_STb], writes=[PB[5]])
                k.op("dve", lambda e: e.tensor_tensor(out=t1[:, :, :], in0=PS[5][:, :].rearrange("p (h d) -> p h d", h=8),
                                                      in1=ex24[:, 0:8].unsqueeze(2).to_broadcast([128, 8, 64]), op=ALU.mult),
                     reads=[PB[5], b_ex], writes=[b_t1])
                k.op("dve", lambda e, s=s: e.tensor_tensor(out=t2[s][:, :, :], in0=t1[:, :, :], in1=PS[4][:, :].rearrange("p (h d) -> p h d", h=8), op=ALU.add),
                     reads=[b_t1, PB[4]], writes=[b_t2[s]])
                t2f = t2[s].ap().rearrange("p h d -> p (h d)")
                if dr == 0:
                    k.op("pool", lambda e, s=s: e.tensor_tensor(out=t2[s][:, :, :], in0=t2[s][:, :, :], in1=xD[:, :, :], op=ALU.add),
                         reads=[b_t2[s], b_xD], writes=[b_t2[s]])
                    k.dma("sp", yf[tok0:tok0 + 128, :], t2f, reads=[b_t2[s]], writes=[b_yf])
                else:
                    k.op("pool", lambda e, s=s, t2f=t2f: e.tensor_tensor(out=t2f, in0=t2f, in1=yfl[s][:, :], op=ALU.add),
                         reads=[b_t2[s], b_yfl[s]], writes=[b_t2[s]])
                k.op("pool", lambda e: e.tensor_tensor(out=xw[:, :, :], in0=xdt[:, :, :], in1=ex24[:, 8:16].unsqueeze(2).to_broadcast([128, 8, 64]), op=ALU.mult),
                     reads=[b_xdt, b_ex], writes=[b_xw])
                xwf = xw.ap().rearrange("p h d -> p (h d)")
                k.op("pe", lambda e, xwf=xwf: e.matmul(PS[6][:, :], lhsT=Btok[:, :], rhs=xwf, start=True, stop=True), reads=[b_Btok, b_xw], writes=[PB[6]])
                k.op("pool", lambda e: e.tensor_tensor(out=ST[:, :, :], in0=ST[:, :, :], in1=ex24[:, 16:24].unsqueeze(2).to_broadcast([128, 8, 64]), op=ALU.mult),
                     reads=[b_ST, b_ex], writes=[b_ST])
                k.op("dve", lambda e: e.tensor_tensor(out=ST[:, :, :], in0=ST[:, :, :], in1=PS[6][:, :].rearrange("p (h d) -> p h d", h=8), op=ALU.add),
                     reads=[b_ST, PB[6]], writes=[b_ST])
                k.op("act", lambda e: e.copy(out=STb[:, :, :], in_=ST[:, :, :]), reads=[b_ST], writes=[b_STb])
                if dr == 1:
                    p7 = PS[7][:, :].bitcast(BF16)
                    for q in range(4):
                        k.op("pe", lambda e, q=q, s=s: e.transpose(out=p7[:, q * 128:(q + 1) * 128], in_=zT[s][:, q, :], identity=idb[:, :]),
                             reads=[b_zT[s], b_id], writes=[PB[7]], sig=(q == 3))
                    k.op("dve", lambda e, t2f=t2f: e.tensor_tensor(out=yz[:, :], in0=t2f, in1=p7[:, 0:512], op=ALU.mult), reads=[b_t2[s], PB[7]], writes=[b_yz])
                    k.op("act", lambda e: e.activation(out=junk[:, :], in_=yz[:, :], func=AF.Square, accum_out=sm[:, 0:1]), reads=[b_yz], writes=[b_j, b_ss])
                    k.op("act", lambda e: e.activation(out=sm[:, 1:2], in_=sm[:, 0:1], func=AF.Sqrt, scale=1.0 / 512.0, bias=EPS), reads=[b_ss], writes=[b_rs])
                    k.op("dve", lambda e: e.reciprocal(out=sm[:, 1:2], in_=sm[:, 1:2]), reads=[b_rs], writes=[b_rs])
                    k.op("dve", lambda e: e.scalar_tensor_tensor(out=yn[:, :], in0=yz[:, :], scalar=sm[:, 1:2], in1=nwb[:, :], op0=ALU.mult, op1=ALU.mult),
                         reads=[b_yz, b_rs, b_nwb], writes=[b_yn])
                    for q in range(4):
                        k.op("pe", lambda e, q=q: e.transpose(out=p7[:, 512 + q * 128:512 + (q + 1) * 128], in_=yn[:, q * 128:(q + 1) * 128], identity=idb[:, :]),
                             reads=[b_yn, b_id], writes=[PB[7]], sig=(q == 3))
                    k.op("act", lambda e, s=s: e.copy(out=ynT[s][:, :, :], in_=p7[:, 512:1024].rearrange("p (q n) -> p q n", q=4)), reads=[PB[7]], writes=[b_ynT[s]])
                    with nc.allow_non_contiguous_dma(reason="256B runs"):
                        k.dma("sp", mxv[:, :, tok0:tok0 + 128], ynT[s][:, :, :], reads=[b_ynT[s]], writes=[mxb])


Prog.phase_ssd = phase_ssd
Prog.ssd_scan = ssd_scan


def build_layers(layers, first, last):
    p = Prog()
    p.phase_init(from_xs=not first)
    p.phase_ada(layers)
    for l in layers:
        p.phase_mod_mix(l)
        if l % 2 == 0:
            p.phase_ssd(l)
            p.phase_outproj(l, "ssd_w_out_%d" % (l // 2), 32)
        else:
            p.phase_fnet(l)
            p.phase_outproj(l, "fnet_w_out_%d" % (l // 2), 16)
        p.phase_mod_moe(l)
        p.phase_route(l)
        p.phase_experts(l)
        p.phase_combine(l)
    if last:
        p.phase_final()
    else:
        p.phase_dump_xs()
    return p


def build_full():
    return build_layers(list(range(DEPTH)), True, True)


LAUNCH_SPLIT = [[0, 1], [2, 3]]


def kernel(**inputs):
    inp = {k_: np.asarray(v) for k_, v in inputs.items()}
    hc = host_consts()
    xs_state = None
    out = None
    for li, layers in enumerate(LAUNCH_SPLIT):
        first, last = li == 0, li == len(LAUNCH_SPLIT) - 1
        p = build_layers(layers, first, last)
        feed = {}
        for name in p.k.inputs:
            feed[name] = xs_state if name == "xs_in" else host_input(name, inp, hc)
        res = run_bass_kernel_spmd(p.nc, [feed], core_ids=[0])
        if last:
            out = np.asarray(res.results[0]["out"], dtype=np.float32)
        else:
            xs_state = np.ascontiguousarray(res.results[0]["xs_out"])
    return out.reshape(1, SEQ, D)
```
